# Optimizing a Trainium2 kernel written in Bass

```python
import math
import jax, jax.numpy as jnp
from jax import lax
import numpy as np

D_MODEL = 2048
BATCH = 2
SEQ = 4096
DEPTH = 1

N_DIFF_HEADS = 8
DIFF_QK_DIM = 64
DIFF_V_DIM = 2 * DIFF_QK_DIM
N_DSA_HEADS = 8
DSA_HEAD_DIM = 128
N_IDX_HEADS = 16
IDX_DIM = 64
INDEX_TOPK = 256
D_FF = ((8 * D_MODEL // 3 + 255) // 256) * 256
ROPE_THETA = 500000.0
ROPE_FRACTION = 4
Q_BLOCK = 128
ADA_CHUNKS = 6
RMS_EPS = 1e-6

DIFF_Q = N_DIFF_HEADS * 2 * DIFF_QK_DIM
DIFF_K = N_DIFF_HEADS * 2 * DIFF_QK_DIM
DIFF_V = N_DIFF_HEADS * DIFF_V_DIM
DSA_Q = N_DSA_HEADS * DSA_HEAD_DIM
DSA_K = DSA_HEAD_DIM
DSA_V = DSA_HEAD_DIM
IDX_Q = N_IDX_HEADS * IDX_DIM
IDX_K = IDX_DIM
IDX_W = N_IDX_HEADS
PROJ_SIZES = (DIFF_Q, DIFF_K, DIFF_V, DSA_Q, DSA_K, DSA_V, IDX_Q, IDX_K, IDX_W)
PROJ_TOTAL = sum(PROJ_SIZES)
MIX_WIDTH = DIFF_V + N_DSA_HEADS * DSA_HEAD_DIM

kernel_name = "hybrid_diffattn_dsa_swiglu_adaln"


def rms_norm(x, g):
    xf = x.astype(jnp.float32)
    y = xf * lax.rsqrt(jnp.mean(xf * xf, axis=-1, keepdims=True) + RMS_EPS)
    return (y * g.astype(jnp.float32)).astype(x.dtype)


def rope_tables(positions, rot_dim):
    inv_freq = ROPE_THETA ** (-(jnp.arange(0, rot_dim, 2, dtype=jnp.float32) / rot_dim))
    ang = positions.astype(jnp.float32)[..., None] * inv_freq
    return jnp.cos(ang), jnp.sin(ang)


def partial_rope(x, positions):
    d = x.shape[-1]
    rot = d // ROPE_FRACTION
    cos, sin = rope_tables(positions, rot)
    cos, sin = cos[:, :, None, :], sin[:, :, None, :]
    xr = x[..., :rot].astype(jnp.float32)
    x1, x2 = xr[..., : rot // 2], xr[..., rot // 2:]
    rotated = jnp.concatenate([x1 * cos - x2 * sin, x2 * cos + x1 * sin], axis=-1)
    return jnp.concatenate([rotated.astype(x.dtype), x[..., rot:]], axis=-1)


def differential_attention(q1, q2, k1, k2, v, lam):
    B, S, H, dq = q1.shape
    scale = dq ** -0.5
    key_pos = jnp.arange(S)

    def block(i):
        start = i * Q_BLOCK
        qb1 = lax.dynamic_slice_in_dim(q1, start, Q_BLOCK, axis=1)
        qb2 = lax.dynamic_slice_in_dim(q2, start, Q_BLOCK, axis=1)
        q_pos = start + jnp.arange(Q_BLOCK)
        mask = key_pos[None, :] <= q_pos[:, None]
        s1 = jnp.einsum('bqhd,bkhd->bhqk', qb1, k1).astype(jnp.float32) * scale
        s2 = jnp.einsum('bqhd,bkhd->bhqk', qb2, k2).astype(jnp.float32) * scale
        p1 = jax.nn.softmax(jnp.where(mask, s1, -jnp.inf), axis=-1)
        p2 = jax.nn.softmax(jnp.where(mask, s2, -jnp.inf), axis=-1)
        attn = p1 - lam * p2
        return jnp.einsum('bhqk,bkhd->bqhd', attn.astype(v.dtype), v)

    out = lax.map(block, jnp.arange(S // Q_BLOCK))
    return out.transpose(1, 0, 2, 3, 4).reshape(B, S, H, v.shape[-1])


def indexed_sparse_attention(q, k, v, iq, ik, iw, k_top):
    B, S, H, D = q.shape
    scale = D ** -0.5
    key_pos = jnp.arange(S)

    def block(i):
        start = i * Q_BLOCK
        qb = lax.dynamic_slice_in_dim(q, start, Q_BLOCK, axis=1)
        iqb = lax.dynamic_slice_in_dim(iq, start, Q_BLOCK, axis=1)
        iwb = lax.dynamic_slice_in_dim(iw, start, Q_BLOCK, axis=1)
        q_pos = start + jnp.arange(Q_BLOCK)
        mask = key_pos[None, :] <= q_pos[:, None]
        logits = jnp.einsum('bqhd,bkd->bqhk', iqb, ik).astype(jnp.float32)
        score = jnp.einsum('bqhk,bqh->bqk', jax.nn.relu(logits), iwb.astype(jnp.float32))
        score = jnp.where(mask[None], score, -jnp.inf)
        _, sel = lax.top_k(score, k_top)
        valid = sel <= q_pos[None, :, None]
        k_sel = jax.vmap(lambda kb, ib: kb[ib])(k, sel)
        v_sel = jax.vmap(lambda vb, ib: vb[ib])(v, sel)
        s = jnp.einsum('bqhd,bqkd->bhqk', qb, k_sel).astype(jnp.float32) * scale
        s = jnp.where(valid[:, None], s, -jnp.inf)
        p = jax.nn.softmax(s, axis=-1)
        return jnp.einsum('bhqk,bqkd->bqhd', p.astype(v.dtype), v_sel)

    out = lax.map(block, jnp.arange(S // Q_BLOCK))
    return out.transpose(1, 0, 2, 3, 4).reshape(B, S, H, D)


def setup_inputs(seed: int = 0) -> dict:
    key = jax.random.key(seed)
    ks = jax.random.split(key, 20)
    f32 = jnp.float32

    def nrm(k, shape, scale):
        return jax.random.normal(k, shape, f32) * scale

    def gain(k, shape):
        return 1.0 + 0.05 * jax.random.normal(k, shape, f32)

    x = jax.random.normal(ks[0], (BATCH, SEQ, D_MODEL), f32)
    c = jax.random.normal(ks[1], (BATCH, D_MODEL), f32)
    offset = jax.random.randint(ks[2], (BATCH, 1), 0, 1024, dtype=jnp.int32)
    positions = (jnp.arange(SEQ, dtype=jnp.int32)[None, :] + offset).astype(jnp.int32)
    return {
        "x": x,
        "c": c,
        "positions": positions,
        "w_ada": nrm(ks[3], (DEPTH, D_MODEL, ADA_CHUNKS * D_MODEL), 0.5 * D_MODEL ** -0.5),
        "b_ada": nrm(ks[4], (DEPTH, ADA_CHUNKS * D_MODEL), 0.02),
        "g_attn_pre": gain(ks[5], (DEPTH, D_MODEL)),
        "g_attn_post": gain(ks[6], (DEPTH, D_MODEL)),
        "g_ffn_pre": gain(ks[7], (DEPTH, D_MODEL)),
        "g_ffn_post": gain(ks[8], (DEPTH, D_MODEL)),
        "w_in": nrm(ks[9], (DEPTH, D_MODEL, PROJ_TOTAL), D_MODEL ** -0.5),
        "lambda_q1": nrm(ks[10], (DEPTH, DIFF_QK_DIM), 0.1),
        "lambda_k1": nrm(ks[11], (DEPTH, DIFF_QK_DIM), 0.1),
        "lambda_q2": nrm(ks[12], (DEPTH, DIFF_QK_DIM), 0.1),
        "lambda_k2": nrm(ks[13], (DEPTH, DIFF_QK_DIM), 0.1),
        "g_diff_sub": gain(ks[14], (DEPTH, DIFF_V_DIM)),
        "w_o": nrm(ks[15], (DEPTH, MIX_WIDTH, D_MODEL), MIX_WIDTH ** -0.5),
        "w_gate": nrm(ks[16], (DEPTH, D_MODEL, D_FF), D_MODEL ** -0.5),
        "w_up": nrm(ks[17], (DEPTH, D_MODEL, D_FF), D_MODEL ** -0.5),
        "w_down": nrm(ks[18], (DEPTH, D_FF, D_MODEL), D_FF ** -0.5),
    }


def reference(x, c, positions, w_ada, b_ada, g_attn_pre, g_attn_post, g_ffn_pre,
              g_ffn_post, w_in, lambda_q1, lambda_k1, lambda_q2, lambda_k2,
              g_diff_sub, w_o, w_gate, w_up, w_down):
    B, S, _ = x.shape
    k_top = min(INDEX_TOPK, S // 4)
    offsets = [0]
    for sz in PROJ_SIZES:
        offsets.append(offsets[-1] + sz)
    idx_w_scale = (N_IDX_HEADS ** -0.5) * (IDX_DIM ** -0.5)

    for l in range(DEPTH):
        mod = jax.nn.silu(c) @ w_ada[l] + b_ada[l]
        shift_a, scale_a, gate_a, shift_f, scale_f, gate_f = jnp.split(mod, ADA_CHUNKS, axis=-1)

        h = rms_norm(x, g_attn_pre[l]) * (1.0 + scale_a[:, None]) + shift_a[:, None]
        proj = h @ w_in[l]
        parts = [proj[..., offsets[i]:offsets[i + 1]] for i in range(len(PROJ_SIZES))]
        d_q, d_k, d_v, s_q, s_k, s_v, i_q, i_k, i_w = parts

        d_q = d_q.reshape(B, S, N_DIFF_HEADS, 2, DIFF_QK_DIM)
        d_k = d_k.reshape(B, S, N_DIFF_HEADS, 2, DIFF_QK_DIM)
        q1 = partial_rope(d_q[..., 0, :], positions)
        q2 = partial_rope(d_q[..., 1, :], positions)
        k1 = partial_rope(d_k[..., 0, :], positions)
        k2 = partial_rope(d_k[..., 1, :], positions)
        d_v = d_v.reshape(B, S, N_DIFF_HEADS, DIFF_V_DIM)
        lam_init = 0.8 - 0.6 * math.exp(-0.3 * l)
        lam = (jnp.exp(jnp.sum(lambda_q1[l].astype(jnp.float32) * lambda_k1[l].astype(jnp.float32)))
               - jnp.exp(jnp.sum(lambda_q2[l].astype(jnp.float32) * lambda_k2[l].astype(jnp.float32)))
               + lam_init)
        diff_out = differential_attention(q1, q2, k1, k2, d_v, lam)
        diff_out = rms_norm(diff_out, g_diff_sub[l]) * (1.0 - lam_init)
        diff_out = diff_out.reshape(B, S, DIFF_V)

        s_q = partial_rope(s_q.reshape(B, S, N_DSA_HEADS, DSA_HEAD_DIM), positions)
        s_k = partial_rope(s_k.reshape(B, S, 1, DSA_HEAD_DIM), positions)[:, :, 0]
        i_q = partial_rope(i_q.reshape(B, S, N_IDX_HEADS, IDX_DIM), positions)
        i_k = partial_rope(i_k.reshape(B, S, 1, IDX_DIM), positions)[:, :, 0]
        i_w = i_w * idx_w_scale
        dsa_out = indexed_sparse_attention(s_q, s_k, s_v, i_q, i_k, i_w, k_top)
        dsa_out = dsa_out.reshape(B, S, N_DSA_HEADS * DSA_HEAD_DIM)

        mixed = jnp.concatenate([diff_out, dsa_out], axis=-1) @ w_o[l]
        x = x + gate_a[:, None] * rms_norm(mixed, g_attn_post[l])

        h = rms_norm(x, g_ffn_pre[l]) * (1.0 + scale_f[:, None]) + shift_f[:, None]
        ff = (jax.nn.silu(h @ w_gate[l]) * (h @ w_up[l])) @ w_down[l]
        x = x + gate_f[:, None] * rms_norm(ff, g_ffn_post[l])

    return x
```

```python
import math
from contextlib import ExitStack

import numpy as np
import ml_dtypes
import concourse.bass as bass
import concourse.mybir as mybir
from concourse.bass_utils import run_bass_kernel_spmd

F32 = mybir.dt.float32
BF16 = mybir.dt.bfloat16
I32 = mybir.dt.int32
ALU = mybir.AluOpType
AF = mybir.ActivationFunctionType
AX = mybir.AxisListType

D = 2048
S = 4096
NT = 32
TOWN = 1024
DFF = 5632
NFF = DFF // 128
KC = 16
PROJ = 5456
O_DQ, O_DK, O_DV, O_SQ, O_SK, O_SV, O_IQ, O_IK, O_IW = 0, 1024, 2048, 3072, 4096, 4224, 4352, 5376, 5440
EPS = 1e-6
THETA = 500000.0
IDX_W_SCALE = (16 ** -0.5) * (64 ** -0.5)
LAM_INIT = 0.8 - 0.6 * math.exp(0.0)
NEG = -1.0e30
NBIS = 12
PI = math.pi

ENGS = ["sync", "scalar", "gpsimd", "vector", "tensor"]


def slot_blocks(r):
    out = []
    for g in range(4):
        out += [8 * g + r, 8 * g + 7 - r]
    return out


def slot_ntiles(i):
    g, hi = divmod(i, 2)
    return 8 * g + (8 if hi else 4)


class Sem:
    def __init__(self, nc, name):
        self.h = nc.alloc_semaphore(name=name)
        self.n = 0


class Ev:
    __slots__ = ("sem", "val")

    def __init__(self, sem=None, val=None):
        self.sem = sem
        self.val = val


class Buf:
    def __init__(self, ap=None):
        self.ap = ap
        self.w = None
        self.r = []


class Prog:
    def __init__(self, nc):
        self.nc = nc
        self.q = {e: [] for e in ENGS}
        self.done = {e: Sem(nc, "done_" + e) for e in ["scalar", "gpsimd", "vector", "tensor"]}
        self.pending = {e: [] for e in ENGS}
        self.dma_pool = [Sem(nc, "dma%d" % i) for i in range(56)]
        self.dma_free = list(self.dma_pool)
        self.all_pending = []

    def get_dma_sems(self, n):
        out = [self.dma_free.pop() for _ in range(n)]
        return out

    def put_dma_sems(self, sems):
        self.dma_free.extend(sems)

    def _emit_wait(self, eng, ev):
        def f(e, st, ev=ev):
            if ev.val is None:
                raise RuntimeError("wait on unresolved event (missing mark) on engine " + eng)
            if ev.val <= 0:
                return
            key = id(ev.sem)
            if st.get(key, 0) >= ev.val:
                return
            st[key] = ev.val
            e.wait_ge(ev.sem.h, ev.val)
        self.q[eng].append(f)

    def _deps(self, eng, reads, writes, extra):
        evs = []
        for b in reads:
            if b.w is not None:
                evs.append(b.w)
        for b in writes:
            if b.w is not None:
                evs.append(b.w)
            evs.extend(b.r)
        evs.extend(extra)
        seen = set()
        for ev in evs:
            if id(ev) in seen:
                continue
            seen.add(id(ev))
            self._emit_wait(eng, ev)

    def op(self, eng, fn, reads=(), writes=(), waits=(), mark=True):
        self._deps(eng, reads, writes, waits)
        if mark:
            sem = self.done[eng]
            sem.n += 1
            ev = Ev(sem, sem.n)
            self.q[eng].append(lambda e, st, fn=fn, sem=sem: fn(e).then_inc(sem.h, 1))
            for pev in self.pending[eng]:
                pev.sem, pev.val = sem, sem.n
            self.pending[eng] = []
        else:
            ev = Ev()
            self.pending[eng].append(ev)
            self.all_pending.append(ev)
            self.q[eng].append(lambda e, st, fn=fn: fn(e))
        for b in reads:
            b.r.append(ev)
        for b in writes:
            b.w = ev
            b.r = []
        return ev

    def dma(self, eng, sem, out, in_, reads=(), writes=(), waits=(), **kw):
        if eng == "gpsimd":
            hist = getattr(self, "_gp_hist", [])
            if len(hist) >= 3:
                self._emit_wait(eng, hist[-3])
            self._gp_hist = hist
        self._deps(eng, reads, writes, waits)
        sem.n += 16
        ev = Ev(sem, sem.n)
        self.q[eng].append(
            lambda e, st, out=out, in_=in_, sem=sem, kw=kw: e.dma_start(out=out, in_=in_, **kw).then_inc(sem.h, 16))
        if eng == "gpsimd":
            self._gp_hist.append(ev)
            self._gp_hist = self._gp_hist[-4:]
        for b in reads:
            b.r.append(ev)
        for b in writes:
            b.w = ev
            b.r = []
        return ev

    def wait(self, eng, ev):
        self._emit_wait(eng, ev)

    def run_block(self, final_waits=()):
        for ev in final_waits:
            self._emit_wait("sync", ev)
        with self.nc.allow_non_contiguous_dma(reason="small strided vector/table layouts"), self.nc.Block() as blk:
            for name in ENGS:
                ops = self.q[name]
                if not ops:
                    continue

                def body(e, ops=ops):
                    st = {}
                    for f in ops:
                        f(e, st)
                getattr(blk, name)(body)
        self.q = {e: [] for e in ENGS}
        for ev in self.all_pending:
            if ev.val is None:
                ev.val = 0
        self.all_pending = []
        self.pending = {e: [] for e in ENGS}


class Ring:
    def __init__(self, P, bufs, with_sems=True):
        self.bufs = bufs
        self.n = len(bufs)
        self.sems = P.get_dma_sems(self.n) if with_sems else None
        self.k = 0
        self.P = P

    def next(self):
        i = self.k % self.n
        self.k += 1
        return self.bufs[i], (self.sems[i] if self.sems else None)

    def release(self):
        if self.sems:
            self.P.put_dma_sems(self.sems)


def build_program(debug=None):
    nc = bass.Bass("TRN2", target_bir_lowering=False)
    dbg = debug or set()

    def din(name, shape, dt=F32):
        if ("tiny" in dbg and name in ("w_ada", "w_o", "w_gate", "w_up", "w_down")) or ("tiny:" + name) in dbg:
            nc.dram_tensor(name, [1, 1], dt, kind="ExternalInput")
            return None
        return nc.dram_tensor(name, list(shape), dt, kind="ExternalInput").ap()

    def dscr(name, shape, dt):
        kind = "ExternalOutput" if (name in dbg) else "Internal"
        return nc.dram_tensor(name, list(shape), dt, kind=kind).ap()

    x_all = din("x_all", [S, D])
    x_own = din("x_own", [TOWN, D])
    pos_all = din("pos_all", [1, S], I32)
    pos_own = din("pos_own", [1, TOWN], I32)
    c_in = din("c_in", [1, D])
    w_ada = din("w_ada", [D, 6 * D])
    b_ada = din("b_ada", [1, 6 * D])
    g_attn_pre = din("g_attn_pre", [1, D])
    g_attn_post = din("g_attn_post", [1, D])
    g_ffn_pre = din("g_ffn_pre", [1, D])
    g_ffn_post = din("g_ffn_post", [1, D])
    w_in = din("w_in", [D, PROJ])
    lam_q1 = din("lambda_q1", [1, 64])
    lam_k1 = din("lambda_k1", [1, 64])
    lam_q2 = din("lambda_q2", [1, 64])
    lam_k2 = din("lambda_k2", [1, 64])
    g_diff_sub = din("g_diff_sub", [1, 128])
    w_o = din("w_o", [D, D])
    w_gate = din("w_gate", [D, DFF])
    w_up = din("w_up", [D, DFF])
    w_down = din("w_down", [DFF, D])
    cst_f32 = din("cst_f32", [128, 8])
    cst_rm = din("cst_rm", [2, 128, 128])
    cst_ident = din("cst_ident", [128, 128])
    cst_dmask = din("cst_dmask", [128, 32])
    cst_bis = din("cst_bis", [128, 2 * NBIS])
    cst_maskT = din("cst_maskT", [128, 32, 128])
    cst_maskadd = din("cst_maskadd", [128, 8, 512])

    out_own = nc.dram_tensor("out_own", [TOWN, D], F32, kind="ExternalOutput").ap()

    mod_scr = dscr("mod_scr", [1, 6 * D], F32)
    kdT_scr = dscr("kdT_scr", [8, 128, S], BF16)
    vd_scr = dscr("vd_scr", [S, 1024], BF16)
    skT_scr = dscr("skT_scr", [128, S], BF16)
    sv_scr = dscr("sv_scr", [S, 128], BF16)
    ikT_scr = dscr("ikT_scr", [64, S], BF16)
    qdT_scr = dscr("qdT_scr", [8, 128, TOWN], BF16)
    sqT_scr = dscr("sqT_scr", [8, 128, TOWN], BF16)
    iqT_scr = dscr("iqT_scr", [8, 128, TOWN], BF16)
    x1_scr = dscr("x1_scr", [TOWN, D], F32)
    ff_scr = dscr("ff_scr", [TOWN, D], F32)
    dbg_scr = dscr("dbg_scr", [128, 4096], F32)

    P = Prog(nc)
    top = ExitStack()

    def sb(name, shape, dt, stack=top):
        return stack.enter_context(nc.sbuf_tensor(name, list(shape), dt))

    def ps(name, shape, dt, stack):
        return stack.enter_context(nc.psum_tensor(name, list(shape), dt))

    cstf = sb("cstf", [128, 8], F32)
    rm_f = sb("rm_f", [128, 2, 128], F32)
    rm_b = sb("rm_b", [128, 2, 128], BF16)
    ident_f = sb("ident_f", [128, 128], F32)
    ident_b = sb("ident_b", [128, 128], BF16)
    ones_b = sb("ones_b", [128, 128], BF16)
    negpi = sb("negpi", [128, 1], F32)
    epsT = sb("epsT", [128, 1], F32)
    halfpi = sb("halfpi", [128, 1], F32)
    B_eps = Buf()
    modA = sb("modA", [128, 4, KC], F32)
    iw_sb = sb("iw_sb", [128, 8, 16], F32)
    B_cstf, B_rmf, B_idf, B_rmb, B_idb, B_ones, B_negpi, B_mod, B_iw = [Buf() for _ in range(9)]

    s0 = P.get_dma_sems(3)
    P.dma("sync", s0[0], cstf[:], cst_f32, writes=[B_cstf])
    P.dma("sync", s0[1], rm_f[:], cst_rm.rearrange("k p m -> p k m"), writes=[B_rmf])
    P.dma("sync", s0[2], ident_f[:], cst_ident, writes=[B_idf])
    P.op("vector", lambda e: e.tensor_copy(out=rm_b[:], in_=rm_f[:]), reads=[B_rmf], writes=[B_rmb])
    P.op("vector", lambda e: e.tensor_copy(out=ident_b[:], in_=ident_f[:]), reads=[B_idf], writes=[B_idb])
    P.op("vector", lambda e: e.memset(ones_b[:], 1.0), writes=[B_ones])
    P.op("vector", lambda e: e.memset(negpi[:], -PI), writes=[B_negpi])
    P.op("vector", lambda e: e.memset(epsT[:], EPS), writes=[B_eps])
    P.op("vector", lambda e: e.memset(halfpi[:], 0.5 * PI), writes=[B_negpi])
    P.run_block()
    P.put_dma_sems(s0)

    if "skipA" in dbg:
        P.op("vector", lambda e: e.memset(modA[:], 0.0), writes=[B_mod])
        P.op("vector", lambda e: e.memset(modA[:, 0, :], 1.0), writes=[B_mod])
        P.op("vector", lambda e: e.memset(modA[:, 2, :], 1.0), writes=[B_mod])
        P.run_block()
    with ExitStack() as st:
      if "skipA" not in dbg:
          cT = sb("cT", [128, KC], F32, st)
          sc = sb("sc", [128, KC], BF16, st)
          brow = sb("brow", [1, 6 * D], F32, st)
          wab = [sb("wab%d" % i, [128, KC, 512], BF16, st) for i in range(3)]
          psa = [ps("psa%d" % i, [128, 512], F32, st) for i in range(2)]
          gv = sb("gv", [128, 2, KC], F32, st)
          mv = sb("mv", [128, 4, KC], F32, st)
          sems = P.get_dma_sems(8)
          Bc, Bsc, Bbrow, Bms = Buf(), Buf(), Buf(), Buf()
          wring = Ring(P, [Buf() for _ in range(3)])
          pring = [Buf(), Buf()]
          crow = sb("crow", [16, 128], F32, st)
          mrow = sb("mrow", [96, 128], F32, st)
          grow = sb("grow", [32, 128], F32, st)
          mvall = sb("mvall", [128, 96], F32, st)
          pst = ps("pst", [128, 128], F32, st)
          Bpst, Bmrow, Bgrow, Bmvall = Buf(), Buf(), Buf(), Buf()
          P.dma("sync", sems[0], crow[:], c_in.rearrange("o (k p) -> (o k) p", p=128), writes=[Bc])
          P.dma("sync", sems[1], brow[:], b_ada, writes=[Bbrow])
          P.op("tensor", lambda e: e.transpose(out=pst[:, 0:16], in_=crow[:], identity=ident_f[0:16, 0:16]),
               reads=[Bc, B_idf], writes=[Bpst], mark=True)
          P.op("scalar", lambda e: e.activation(out=sc[:], in_=pst[:, 0:16], func=AF.Silu), reads=[Bpst], writes=[Bsc])
          w_ada_v = w_ada.rearrange("(k p) n -> p k n", p=128)
          for j in range(24):
              wb, wsem = wring.next()
              i = (j % 3)
              P.dma("gpsimd", wsem, wab[i][:], w_ada_v[:, :, j * 512:(j + 1) * 512], writes=[wb])
              pb = pring[j % 2]
              for k in range(KC):
                  P.op("tensor",
                       lambda e, i=i, k=k, j=j: e.matmul(psa[j % 2][0:1, :], lhsT=sc[:, k:k + 1], rhs=wab[i][:, k, :],
                                                         start=(k == 0), stop=(k == KC - 1)),
                       reads=[Bsc, wb], writes=[pb] if k == 0 else [], mark=(k == KC - 1))
              P.op("vector",
                   lambda e, j=j: e.tensor_tensor(out=brow[0:1, j * 512:(j + 1) * 512], in0=psa[j % 2][0:1, :],
                                                  in1=brow[0:1, j * 512:(j + 1) * 512], op=ALU.add),
                   reads=[pb], writes=[Bbrow])
          P.dma("sync", sems[2], mod_scr, brow[:], reads=[Bbrow], writes=[Bms])
          P.dma("sync", sems[3], mrow[:], mod_scr.rearrange("o (r p) -> (o r) p", p=128), reads=[Bms], writes=[Bmrow])
          P.dma("sync", sems[4], grow[0:16, :], g_attn_pre.rearrange("o (k p) -> (o k) p", p=128), writes=[Bgrow])
          P.dma("sync", sems[5], grow[16:32, :], g_ffn_pre.rearrange("o (k p) -> (o k) p", p=128), writes=[Bgrow])
          gev = [Ev(sems[4], sems[4].n), Ev(sems[5], sems[5].n)]
          P.op("tensor", lambda e: e.transpose(out=pst[:, 0:96], in_=mrow[:], identity=ident_f[0:96, 0:96]),
               reads=[Bmrow, B_idf], writes=[Bpst], mark=True)
          P.op("vector", lambda e: e.tensor_copy(out=mvall[:], in_=pst[:, 0:96]), reads=[Bpst], writes=[Bmvall])
          P.op("tensor", lambda e: e.transpose(out=pst[:, 0:32], in_=grow[:], identity=ident_f[0:32, 0:32]),
               reads=[Bgrow, B_idf], writes=[Bpst], waits=gev, mark=True)
          Bgv2 = Buf()
          P.op("vector", lambda e: e.tensor_copy(out=gv[:].rearrange("p a k -> p (a k)"), in_=pst[:, 0:32]), reads=[Bpst], writes=[Bgv2])
          P.op("vector", lambda e: e.scalar_tensor_tensor(out=modA[:, 0, :], in0=mvall[:, 16:32], scalar=1.0, in1=gv[:, 0, :],
                                                          op0=ALU.add, op1=ALU.mult),
               reads=[Bmvall, Bgv2], writes=[B_mod])
          P.op("vector", lambda e: e.tensor_copy(out=modA[:, 1, :], in_=mvall[:, 0:16]), reads=[Bmvall], writes=[B_mod])
          P.op("vector", lambda e: e.scalar_tensor_tensor(out=modA[:, 2, :], in0=mvall[:, 64:80], scalar=1.0, in1=gv[:, 1, :],
                                                          op0=ALU.add, op1=ALU.mult),
               reads=[Bmvall, Bgv2], writes=[B_mod])
          P.op("vector", lambda e: e.tensor_copy(out=modA[:, 3, :], in_=mvall[:, 48:64]), reads=[Bmvall], writes=[B_mod])
          P.run_block()
          wring.release()
          P.put_dma_sems(sems)

    if "stopA" in dbg:
        top.close()
        return nc

    w_in_v = w_in.rearrange("(k p) n -> p k n", p=128)

    def project_set(tag, x_src, pos_src, wblocks):
        T = 1024
        with ExitStack() as st:
            hT = sb("hT" + tag, [128, KC, T], BF16, st)
            tabs = [sb("tab%d%s" % (i, tag), [128, T], F32, st) for i in range(4)]
            xt = [sb("xt%d%s" % (i, tag), [128, D], F32, st) for i in range(2)]
            xs = [sb("xs%d%s" % (i, tag), [128, D], BF16, st) for i in range(2)]
            junk = sb("junk" + tag, [128, D], BF16, st)
            ss = sb("ss" + tag, [128, 8], F32, st)
            rstd = sb("rstd" + tag, [128, 8], F32, st)
            wbuf = [sb("wb%d%s" % (i, tag), [128, KC, 512], BF16, st) for i in range(2)]
            pbs = [sb("pb%d%s" % (i, tag), [128, 512], BF16, st) for i in range(2)]
            t1s = [sb("t1%d%s" % (i, tag), [128, 512], BF16, st) for i in range(2)]
            t2s = [sb("t2%d%s" % (i, tag), [128, 512], BF16, st) for i in range(2)]
            stg = [sb("stg%d%s" % (i, tag), [128, 512], BF16, st) for i in range(3)]
            tp = [ps("tp%d%s" % (i, tag), [128, KC, 128], BF16, st) for i in range(2)]
            p1 = [ps("p1%d%s" % (i, tag), [128, 512], F32, st) for i in range(2)]
            p2 = [ps("p2%d%s" % (i, tag), [128, 512], F32, st) for i in range(2)]
            BhT = [Buf() for _ in range(8)]
            Btab = [Buf() for _ in range(4)]
            Bss, Brstd = [Buf() for _ in range(8)], [Buf() for _ in range(8)]
            Bjunk = Buf()
            xring = Ring(P, [Buf(), Buf()])
            Bxs = [Buf(), Buf()]
            Btp = [Buf(), Buf()]
            wring = Ring(P, [Buf(), Buf()])
            Bp1, Bp2, Bpb, Bt1, Bt2 = ([Buf(), Buf()] for _ in range(5))
            sring = Ring(P, [Buf(), Buf(), Buf()])
            psem = P.get_dma_sems(1)

            if True:
                posi = sb("posi" + tag, [128, T], I32, st)
                posf = sb("posf" + tag, [128, T], F32, st)
                ang = sb("ang" + tag, [128, T], F32, st)
                kf = sb("kf" + tag, [128, T], F32, st)
                Bposi, Bposf, Bang, Bkf = Buf(), Buf(), Buf(), Buf()
                P.dma("sync", psem[0], posi[:], pos_src.to_broadcast([128, T]), writes=[Bposi])
                P.op("vector", lambda e: e.tensor_copy(out=posf[:], in_=posi[:]), reads=[Bposi], writes=[Bposf])
                C1 = 6.28125
                C2 = float(np.float32(2.0 * PI - 6.28125))
                PIC = 3.1415925
                for kd in range(2):
                    P.op("vector", lambda e, kd=kd: e.tensor_scalar(
                        out=ang[:], in0=posf[:], scalar1=cstf[:, kd:kd + 1], scalar2=None, op0=ALU.mult),
                        reads=[Bposf, B_cstf], writes=[Bang])
                    P.op("vector", lambda e: e.tensor_scalar(
                        out=kf[:], in0=ang[:], scalar1=1.0 / (2.0 * PI), scalar2=None, op0=ALU.mult),
                        reads=[Bang], writes=[Bkf])
                    P.op("vector", lambda e: e.tensor_copy(out=posi[:], in_=kf[:]), reads=[Bkf, Bposf], writes=[Bposi])
                    P.op("vector", lambda e: e.tensor_copy(out=kf[:], in_=posi[:]), reads=[Bposi], writes=[Bkf])
                    P.op("vector", lambda e: e.scalar_tensor_tensor(
                        out=ang[:], in0=kf[:], scalar=-C1, in1=ang[:], op0=ALU.mult, op1=ALU.add),
                        reads=[Bkf, Bang], writes=[Bang])
                    P.op("vector", lambda e: e.scalar_tensor_tensor(
                        out=ang[:], in0=kf[:], scalar=-C2, in1=ang[:], op0=ALU.mult, op1=ALU.add),
                        reads=[Bkf, Bang], writes=[Bang])
                    P.op("vector", lambda e: e.tensor_scalar(
                        out=ang[:], in0=ang[:], scalar1=PIC, scalar2=-PIC, op0=ALU.min, op1=ALU.max),
                        reads=[Bang], writes=[Bang])
                    P.op("scalar", lambda e, kd=kd: e.activation(out=tabs[kd * 2 + 1][:], in_=ang[:], func=AF.Sin),
                         reads=[Bang], writes=[Btab[kd * 2 + 1]])
                    P.op("vector", lambda e: e.scalar_tensor_tensor(
                        out=kf[:], in0=ang[:], scalar=-1.0, in1=ang[:], op0=ALU.mult, op1=ALU.min),
                        reads=[Bang], writes=[Bkf])
                    P.op("scalar", lambda e, kd=kd: e.activation(out=tabs[kd * 2][:], in_=kf[:], func=AF.Sin, bias=halfpi[:]),
                         reads=[Bkf, B_negpi], writes=[Btab[kd * 2]])
                P.op("vector", lambda e: e.memset(ss[:], 0.0), reads=Btab, writes=Bss)

            if "stopT" in dbg:
                P.run_block()
                return
            def emit_load(tt):
                xb, xsem = xring.next()
                P.dma("sync", xsem, xt[tt % 2][:], x_src[tt * 128:(tt + 1) * 128, :], writes=[xb])
                return xb

            xb_next = emit_load(0)
            for tt in range(8):
                xb = xb_next
                if tt + 1 < 8:
                    xb_next = emit_load(tt + 1)
                b2 = tt % 2
                P.op("scalar", lambda e, tt=tt, b2=b2: e.activation(
                    out=junk[:], in_=xt[b2][:], func=AF.Square, accum_out=ss[:, tt:tt + 1]),
                    reads=[xb], writes=[Bjunk, Bss[tt]])
                P.op("scalar", lambda e, tt=tt: e.activation(
                    out=rstd[:, tt:tt + 1], in_=ss[:, tt:tt + 1], func=AF.Sqrt, scale=1.0 / D, bias=epsT[:]),
                    reads=[Bss[tt], B_eps], writes=[Brstd[tt]])
                P.op("vector", lambda e, tt=tt: e.reciprocal(out=rstd[:, tt:tt + 1], in_=rstd[:, tt:tt + 1]),
                    reads=[Brstd[tt]], writes=[Brstd[tt]])
                P.op("vector", lambda e, tt=tt, b2=b2: e.tensor_scalar(
                    out=xs[b2][:], in0=xt[b2][:], scalar1=rstd[:, tt:tt + 1], scalar2=None, op0=ALU.mult),
                    reads=[xb, Brstd[tt]], writes=[Bxs[b2]])
                for k in range(KC):
                    P.op("tensor", lambda e, b2=b2, k=k: e.transpose(
                        out=tp[b2][:, k, :], in_=xs[b2][:, k * 128:(k + 1) * 128], identity=ident_b[:]),
                        reads=[Bxs[b2], B_idb], writes=[Btp[b2]] if k == 0 else [], mark=(k == KC - 1))
                for k in range(KC):
                    P.op("scalar", lambda e, b2=b2, k=k, tt=tt: e.activation(
                        out=hT[:, k, tt * 128:(tt + 1) * 128], in_=tp[b2][:, k, :], func=AF.Identity,
                        scale=modA[:, 0, k:k + 1], bias=modA[:, 1, k:k + 1]),
                        reads=[Btp[b2], B_mod], writes=[BhT[tt]] if k == 0 else [], mark=True)
                BhT[tt].w = Ev(P.done["scalar"], P.done["scalar"].n)

            if "stopS1" in dbg:
                P.run_block()
                return
            items = []
            for (c0, ncols, jobs) in wblocks:
                for tc in range(2):
                    for job in jobs:
                        if job[0] == "fm":
                            items.append((c0, ncols, job, tc, None))
                        else:
                            for t4 in range(4):
                                items.append((c0, ncols, job, tc, t4))
            cur_w = {"key": None, "buf": None, "idx": -1}
            loaded = {}
            wcount = [0]

            def ensure_w(c0, ncols):
                if c0 not in loaded:
                    wb, wsem = wring.next()
                    i = wcount[0] % 2
                    wcount[0] += 1
                    P.dma("gpsimd", wsem, wbuf[i][:, :, 0:ncols], w_in_v[:, :, c0:c0 + ncols], writes=[wb])
                    loaded[c0] = (wb, i)
                return loaded[c0]

            order = [b[0] for b in wblocks]
            npipe = [0]

            def main(item):
                c0, ncols, job, tc, t4 = item
                wb, wi = ensure_w(c0, ncols)
                bi = order.index(c0)
                if bi + 1 < len(order):
                    nb = wblocks[bi + 1]
                    ensure_w(nb[0], nb[1])
                n = npipe[0]
                npipe[0] += 1
                b2 = n % 2
                rec = {"b2": b2, "item": item}
                if job[0] == "fm":
                    _, off, M, kind, dst = job
                    hbufs = BhT[tc * 4:(tc + 1) * 4]
                    for k in range(KC):
                        P.op("tensor", lambda e, b2=b2, wi=wi, k=k, off=off, M=M, tc=tc: e.matmul(
                            p1[b2][0:M, :], lhsT=wbuf[wi][:, k, off:off + M], rhs=hT[:, k, tc * 512:(tc + 1) * 512],
                            start=(k == 0), stop=(k == KC - 1)),
                            reads=[wb] + hbufs, writes=[Bp1[b2]] if k == 0 else [], mark=(k == KC - 1))
                else:
                    off = job[1]
                    N = 16 if job[0] == "iw" else job[2]
                    tt = tc * 4 + t4
                    for k in range(KC):
                        P.op("tensor", lambda e, b2=b2, wi=wi, k=k, off=off, N=N, tt=tt: e.matmul(
                            p1[b2][:, 0:N], lhsT=hT[:, k, tt * 128:(tt + 1) * 128], rhs=wbuf[wi][:, k, off:off + N],
                            start=(k == 0), stop=(k == KC - 1)),
                            reads=[wb, BhT[tt]], writes=[Bp1[b2]] if k == 0 else [], mark=(k == KC - 1))
                return rec

            def tail(rec):
                b2 = rec["b2"]
                c0, ncols, job, tc, t4 = rec["item"]
                if job[0] == "fm":
                    _, off, M, kind, dst = job
                    P.op("vector", lambda e, b2=b2, M=M, kind=kind, tc=tc: e.tensor_tensor(
                        out=t1s[b2][0:M, :], in0=p1[b2][0:M, :], in1=tabs[kind * 2][0:M, tc * 512:(tc + 1) * 512],
                        op=ALU.mult), reads=[Bp1[b2], Btab[kind * 2]], writes=[Bt1[b2]])
                    P.op("vector", lambda e, b2=b2, M=M, kind=kind, tc=tc: e.tensor_tensor(
                        out=t2s[b2][0:M, :], in0=p1[b2][0:M, :], in1=tabs[kind * 2 + 1][0:M, tc * 512:(tc + 1) * 512],
                        op=ALU.mult), reads=[Bp1[b2], Btab[kind * 2 + 1]], writes=[Bt2[b2]])
                    P.op("tensor", lambda e, b2=b2, M=M: e.matmul(
                        p2[b2][0:M, :], lhsT=ident_b[0:M, 0:M], rhs=t1s[b2][0:M, :], start=True, stop=False),
                        reads=[Bt1[b2], B_idb], writes=[Bp2[b2]], mark=False)
                    P.op("tensor", lambda e, b2=b2, M=M, kind=kind: e.matmul(
                        p2[b2][0:M, :], lhsT=rm_b[0:M, kind, 0:M], rhs=t2s[b2][0:M, :], start=False, stop=True),
                        reads=[Bt2[b2], B_rmb], writes=[], mark=True)
                    sbuf_, ssem = sring.next()
                    si = (sring.k - 1) % 3
                    P.op("scalar", lambda e, b2=b2, M=M, si=si: e.activation(
                        out=stg[si][0:M, :], in_=p2[b2][0:M, :], func=AF.Copy),
                        reads=[Bp2[b2]], writes=[sbuf_])
                    P.dma("sync", ssem, dst[:, tc * 512:(tc + 1) * 512], stg[si][0:M, :], reads=[sbuf_])
                elif job[0] == "tm":
                    _, off, N, dst = job
                    tt = tc * 4 + t4
                    sbuf_, ssem = sring.next()
                    si = (sring.k - 1) % 3
                    P.op("scalar", lambda e, b2=b2, N=N, si=si: e.activation(
                        out=stg[si][:, 0:N], in_=p1[b2][:, 0:N], func=AF.Copy),
                        reads=[Bp1[b2]], writes=[sbuf_])
                    P.dma("sync", ssem, dst[tt * 128:(tt + 1) * 128, :], stg[si][:, 0:N], reads=[sbuf_])
                else:
                    tt = tc * 4 + t4
                    P.op("scalar", lambda e, b2=b2, tt=tt: e.activation(
                        out=iw_sb[:, tt, :], in_=p1[b2][:, 0:16], func=AF.Copy, scale=float(IDX_W_SCALE)),
                        reads=[Bp1[b2]], writes=[B_iw])

            prev = None
            for item in items:
                rec = main(item)
                if prev is not None:
                    tail(prev)
                prev = rec
            tail(prev)
            tails = [Ev(s, s.n) for s in sring.sems]
            P.run_block(final_waits=tails)
            xring.release()
            wring.release()
            sring.release()
            P.put_dma_sems(psem)

    for s_ in range(4):
        t0 = s_ * 1024
        blocks = []
        for hb in range(2):
            jobs = [("fm", hh * 128, 128, 0, kdT_scr[hb * 4 + hh][:, t0:t0 + 1024]) for hh in range(4)]
            blocks.append((O_DK + hb * 512, 512, jobs))
        for cb in range(2):
            blocks.append((O_DV + cb * 512, 512, [("tm", 0, 512, vd_scr[t0:t0 + 1024, cb * 512:(cb + 1) * 512])]))
        blocks.append((O_SK, 256, [("fm", 0, 128, 1, skT_scr[:, t0:t0 + 1024]),
                                   ("tm", 128, 128, sv_scr[t0:t0 + 1024, :])]))
        blocks.append((O_IK, 64, [("fm", 0, 64, 0, ikT_scr[:, t0:t0 + 1024])]))
        for d_ in dbg:
            if d_.startswith("blk:"):
                blocks = [blocks[int(v)] for v in d_[4:].split(",")]
        project_set("k%d" % s_, x_all[t0:t0 + 1024, :], pos_all[0:1, t0:t0 + 1024], blocks)
        if "stopB1" in dbg:
            top.close()
            return nc
    blocks = []
    for hb in range(2):
        blocks.append((O_DQ + hb * 512, 512, [("fm", hh * 128, 128, 0, qdT_scr[hb * 4 + hh]) for hh in range(4)]))
    for hb in range(2):
        blocks.append((O_SQ + hb * 512, 512, [("fm", hh * 128, 128, 1, sqT_scr[hb * 4 + hh]) for hh in range(4)]))
    for hb in range(2):
        blocks.append((O_IQ + hb * 512, 512, [("fm", hh * 128, 128, 0, iqT_scr[hb * 4 + hh]) for hh in range(4)]))
    blocks.append((O_IW, 16, [("iw", 0)]))
    project_set("q", x_own, pos_own, blocks)
    if "stopB" in dbg:
        top.close()
        return nc

    NTL = [slot_ntiles(i) for i in range(8)]
    TBASE = [sum(NTL[:i]) for i in range(8)]
    attn_stack = ExitStack()
    mask_stack = ExitStack()
    mixT = sb("mixT", [128, 16, TOWN], BF16, attn_stack)
    B_mix = [Buf() for _ in range(16)]
    maskT_sb = sb("maskT_sb", [128, 144, 128], BF16, mask_stack)
    B_maskT = [Buf() for _ in range(8)]

    with ExitStack() as st:
        IQr = sb("IQr", [128, 8 * 4 * 4 * 128], BF16, st)
        iqst = [sb("iqst%d" % i, [64, TOWN], BF16, st) for i in range(2)]
        ikT = sb("ikT", [128, S], BF16, st)
        dmask = sb("dmask", [128, 32], F32, st)
        bisc = sb("bisc", [128, 2 * NBIS], F32, st)
        maskadd = sb("maskadd", [128, 8, 512], F32, st)
        WT = [sb("WT%d" % i, [128, 16, 32], BF16, st) for i in range(2)]
        wselM = [sb("wselM%d" % i, [128, 4, 4, 128], BF16, st) for i in range(2)]
        Rb = [sb("Rb%d" % i, [128, 512], BF16, st) for i in range(3)]
        scores = [sb("scores%d" % i, [128, S], F32, st) for i in range(2)]
        mask_sb = sb("mask_sb", [128, S], BF16, st)
        junk = mask_sb
        sm = sb("smallc", [128, 16], F32, st)
        wtab = sb("wtab", [128, 2 * NBIS], F32, st)
        wsel_ps = ps("wsel_ps", [128, 4, 128], BF16, st)
        L_ps = [ps("L_ps%d" % i, [128, 512], F32, st) for i in range(3)]
        sc_ps = [ps("sc_ps%d" % i, [128, 512], F32, st) for i in range(2)]
        mt_ps = ps("mt_ps", [128, 4, 128], BF16, st)
        sems = P.get_dma_sems(6)
        BIQ, Bik, Bdm, Bbis, Bmadd = [Buf() for _ in range(5)]
        Bik0 = Buf()
        BWT, BwA = [Buf(), Buf()], [Buf(), Buf()]
        Bwps, Bmtps = Buf(), Buf()
        BR, BL = [Buf() for _ in range(3)], [Buf() for _ in range(3)]
        Bsc, Bscore = [Buf(), Buf()], [Buf(), Buf()]
        Bjunk, Bmask, Bsm, Bwtab = Buf(), Buf(), Buf(), Buf()
        iqring = Ring(P, [Buf(), Buf()])
        IQr_v = IQr[0:64, :].rearrange("d (i j a h q) -> d i j a h q", i=8, j=4, a=4, h=4, q=32)
        P.op("vector", lambda e: e.memset(IQr[64:128, :], 0.0), writes=[BIQ])
        P.op("vector", lambda e: e.memset(ikT[64:128, :], 0.0), writes=[Bik0])
        for w_ in range(2):
            P.op("vector", lambda e, w_=w_: e.memset(wselM[w_][:], 0.0), writes=[BwA[w_]])
        for pr in range(8):
            for hp in range(2):
                hd = 2 * pr + hp
                ib, isem = iqring.next()
                P.dma("sync", isem, iqst[hd % 2][:], iqT_scr[pr, hp * 64:(hp + 1) * 64, :], writes=[ib])
                P.op("vector", lambda e, hd=hd: e.tensor_copy(
                    out=IQr_v[:, :, :, hd // 4, hd % 4, :],
                    in_=iqst[hd % 2][:].rearrange("d (i j q) -> d i j q", i=8, j=4, q=32)),
                    reads=[ib], writes=[BIQ])
        iq_evs = []
        P.dma("sync", sems[2], ikT[0:64, :], ikT_scr, writes=[Bik])
        P.dma("sync", sems[3], dmask[:], cst_dmask, writes=[Bdm])
        P.dma("sync", sems[4], bisc[:], cst_bis, writes=[Bbis])
        P.dma("sync", sems[5], maskadd[:], cst_maskadd, writes=[Bmadd])

        for i in range(8):
            n = NTL[i]
            nch = n // 4
            sbi = i % 2
            P.op("vector", lambda e, i=i, sbi=sbi: e.tensor_tensor(
                out=WT[sbi][:], in0=dmask[:].unsqueeze(1).to_broadcast([128, 16, 32]),
                in1=iw_sb[:, i, :].unsqueeze(2).to_broadcast([128, 16, 32]), op=ALU.mult),
                reads=[Bdm, B_iw], writes=[BWT[sbi]])
            for a in range(4):
                P.op("tensor", lambda e, a=a, sbi=sbi: e.transpose(
                    out=wsel_ps[:, a, :], in_=WT[sbi][:, 4 * a:4 * a + 4, :], identity=ident_b[:]),
                    reads=[BWT[sbi], B_idb], writes=[Bwps] if a == 0 else [], mark=(a == 3))
            for j in range(4):
                P.op("scalar", lambda e, sbi=sbi, j=j: e.activation(
                    out=wselM[sbi][:, j, :, 32 * j:32 * j + 32], in_=wsel_ps[:, :, 32 * j:32 * j + 32], func=AF.Copy),
                    reads=[Bwps], writes=[BwA[sbi]])
            cnt = [0]

            def Lmm(c, j, a, i=i):
                lb = cnt[0] % 3
                cnt[0] += 1
                P.op("tensor", lambda e, lb=lb, c=c, j=j, a=a, i=i: e.matmul(
                    L_ps[lb][:], lhsT=IQr[:, ((i * 4 + j) * 4 + a) * 128:((i * 4 + j) * 4 + a) * 128 + 128],
                    rhs=ikT[:, c * 512:(c + 1) * 512], start=True, stop=True),
                    reads=[BIQ, Bik, Bik0], writes=[BL[lb]], waits=iq_evs, mark=True)
                P.op("scalar", lambda e, lb=lb: e.activation(out=Rb[lb][:], in_=L_ps[lb][:], func=AF.Relu),
                     reads=[BL[lb]], writes=[BR[lb]])
                return lb

            def Smm(c, j, a, lb, sp, sbi=sbi):
                P.op("tensor", lambda e, lb=lb, j=j, a=a, sp=sp, sbi=sbi: e.matmul(
                    sc_ps[sp][:], lhsT=wselM[sbi][:, j, a, :], rhs=Rb[lb][:],
                    start=(j == 0 and a == 0), stop=(j == 3 and a == 3)),
                    reads=[BR[lb], BwA[sbi]], writes=[Bsc[sp]] if (j == 0 and a == 0) else [],
                    mark=True)

            for c in range(nch):
                sp = c % 2
                work = [(j, a) for j in range(4) for a in range(4)]
                prev = None
                for (j, a) in work:
                    lb = Lmm(c, j, a)
                    if prev is not None:
                        Smm(c, prev[0], prev[1], prev[2], sp)
                    prev = (j, a, lb)
                Smm(c, prev[0], prev[1], prev[2], sp)
                Bsc[sp].w = Ev(P.done["tensor"], P.done["tensor"].n)
                P.op("scalar", lambda e, sp=sp, c=c, sbi=sbi: e.activation(
                    out=scores[sbi][:, c * 512:(c + 1) * 512], in_=sc_ps[sp][:], func=AF.Copy),
                    reads=[Bsc[sp]], writes=[Bscore[sbi]])
            W = n * 128
            P.op("vector", lambda e, sbi=sbi, W=W: e.tensor_reduce(out=sm[:, 1:2], in_=scores[sbi][:, 0:W], axis=AX.X, op=ALU.max),
                 reads=[Bscore[sbi]], writes=[Bsm])
            P.op("vector", lambda e, sbi=sbi, W=W: e.tensor_reduce(out=sm[:, 0:1], in_=scores[sbi][:, 0:W], axis=AX.X, op=ALU.min),
                 reads=[Bscore[sbi]], writes=[Bsm])
            P.op("vector", lambda e, sbi=sbi, W=W, i=i: e.tensor_tensor(
                out=scores[sbi][:, W - 512:W], in0=scores[sbi][:, W - 512:W], in1=maskadd[:, i, :], op=ALU.add),
                reads=[Bmadd, Bscore[sbi]], writes=[Bscore[sbi]])
            P.op("vector", lambda e: e.tensor_tensor(out=sm[:, 2:3], in0=sm[:, 1:2], in1=sm[:, 0:1], op=ALU.subtract),
                 reads=[Bsm], writes=[Bsm])
            P.op("vector", lambda e: e.tensor_scalar(out=wtab[:], in0=bisc[:], scalar1=sm[:, 2:3], scalar2=None, op0=ALU.mult),
                 reads=[Bsm, Bbis], writes=[Bwtab])
            P.op("vector", lambda e: e.tensor_tensor(out=sm[:, 3:4], in0=sm[:, 0:1], in1=wtab[:, 0:1], op=ALU.add),
                 reads=[Bsm, Bwtab], writes=[Bsm])
            for it in range(NBIS):
                P.op("vector", lambda e, sbi=sbi, W=W: e.tensor_scalar(
                    out=junk[:, 0:W], in0=scores[sbi][:, 0:W], scalar1=sm[:, 3:4], scalar2=0.0,
                    op0=ALU.is_ge, op1=ALU.add, accum_out=sm[:, 4:5]),
                    reads=[Bscore[sbi], Bsm], writes=[Bmask, Bsm])
                P.op("vector", lambda e, it=it: e.scalar_tensor_tensor(
                    out=sm[:, 5:6], in0=sm[:, 4:5], scalar=255.5, in1=wtab[:, it:it + 1], op0=ALU.is_ge, op1=ALU.mult),
                    reads=[Bsm, Bwtab], writes=[Bsm])
                P.op("vector", lambda e, it=it: e.scalar_tensor_tensor(
                    out=sm[:, 3:4], in0=sm[:, 5:6], scalar=wtab[:, NBIS + it:NBIS + it + 1], in1=sm[:, 3:4],
                    op0=ALU.subtract, op1=ALU.add),
                    reads=[Bsm, Bwtab], writes=[Bsm])
            P.op("vector", lambda e, sbi=sbi, W=W: e.tensor_scalar(
                out=mask_sb[:, 0:W], in0=scores[sbi][:, 0:W], scalar1=sm[:, 3:4], scalar2=None, op0=ALU.is_ge),
                reads=[Bscore[sbi], Bsm], writes=[Bmask])
            for g4 in range(nch):
                for t4 in range(4):
                    kt = g4 * 4 + t4
                    P.op("tensor", lambda e, kt=kt, t4=t4: e.transpose(
                        out=mt_ps[:, t4, :], in_=mask_sb[:, kt * 128:(kt + 1) * 128], identity=ident_b[:]),
                        reads=[Bmask, B_idb], writes=[Bmtps] if t4 == 0 else [], mark=(t4 == 3))
                P.op("scalar", lambda e, g4=g4, i=i: e.activation(
                    out=maskT_sb[:, TBASE[i] + g4 * 4:TBASE[i] + g4 * 4 + 4, :], in_=mt_ps[:], func=AF.Copy),
                    reads=[Bmtps], writes=[B_maskT[i]])
        if "dbgC" in dbg:
            dsem = P.get_dma_sems(1)
            P.dma("sync", dsem[0], dbg_scr[:, 0:4096], scores[1][:], reads=[Bscore[1]])
            P.run_block(final_waits=[Ev(dsem[0], dsem[0].n)])
        else:
            P.run_block()
        P.put_dma_sems(sems)
    if "stopC" in dbg:
        mask_stack.close(); attn_stack.close(); top.close()
        return nc

    with ExitStack() as st:
        sqT = sb("sqT", [128, 8, TOWN], BF16, st)
        skT = sb("skT", [128, S], BF16, st)
        svv = sb("svv", [128, NT, 128], BF16, st)
        Eb = [sb("Eb%d" % i, [128, 512], BF16, st) for i in range(3)]
        Pm = [sb("Pm%d" % i, [128, 512], BF16, st) for i in range(3)]
        lnb = sb("lnb", [128, 512], F32, st)
        rinv = sb("rinvd", [128, 512], F32, st)
        st_ps = [ps("st_ps%d" % i, [128, 512], F32, st) for i in range(3)]
        O_ps = [ps("O_ps%d" % i, [128, 512], F32, st) for i in range(2)]
        s_ps = [ps("s_ps%d" % i, [128, 512], F32, st) for i in range(2)]
        sems = P.get_dma_sems(10)
        Bsq = [Buf() for _ in range(8)]
        Bsk, Bsv = Buf(), Buf()
        BE, BPm, Bst = ([Buf() for _ in range(3)] for _ in range(3))
        BO, BS = [Buf(), Buf()], [Buf(), Buf()]
        Bln, Brinv = Buf(), Buf()
        for h in range(8):
            P.dma("sync", sems[h], sqT[:, h, :], sqT_scr[h], writes=[Bsq[h]])
        P.dma("sync", sems[8], skT[:], skT_scr, writes=[Bsk])
        P.dma("gpsimd", sems[9], svv[:], sv_scr.rearrange("(t p) d -> p t d", p=128), writes=[Bsv])
        SC = 128 ** -0.5
        cnt = [0]
        grp = 0
        for i in range(8):
            n = NTL[i]
            for hh in range(2):
                ob = grp % 2
                grp += 1

                def qk(kt, i=i, hh=hh):
                    b3 = cnt[0] % 3
                    cnt[0] += 1
                    P.op("tensor", lambda e, b3=b3, kt=kt, i=i, hh=hh: e.matmul(
                        st_ps[b3][:], lhsT=skT[:, kt * 128:(kt + 1) * 128],
                        rhs=sqT[:, 4 * hh:4 * hh + 4, i * 128:(i + 1) * 128], start=True, stop=True),
                        reads=[Bsk] + Bsq[4 * hh:4 * hh + 4], writes=[Bst[b3]], mark=True)
                    P.op("scalar", lambda e, b3=b3: e.activation(out=Eb[b3][:], in_=st_ps[b3][:], func=AF.Exp, scale=SC),
                         reads=[Bst[b3]], writes=[BE[b3]])
                    P.op("vector", lambda e, b3=b3, kt=kt, i=i: e.tensor_tensor(
                        out=Pm[b3][:].rearrange("p (h q) -> p h q", h=4),
                        in0=Eb[b3][:].rearrange("p (h q) -> p h q", h=4),
                        in1=maskT_sb[:, TBASE[i] + kt, :].unsqueeze(1).to_broadcast([128, 4, 128]), op=ALU.mult),
                        reads=[BE[b3], B_maskT[i]], writes=[BPm[b3]])
                    return b3

                def pv(kt, b3, ob=ob, n=n):
                    P.op("tensor", lambda e, b3=b3, kt=kt, ob=ob, n=n: e.matmul(
                        O_ps[ob][:], lhsT=svv[:, kt, :], rhs=Pm[b3][:], start=(kt == 0), stop=(kt == n - 1)),
                        reads=[Bsv, BPm[b3]], writes=[BO[ob]] if kt == 0 else [], mark=False)
                    P.op("tensor", lambda e, b3=b3, kt=kt, ob=ob, n=n: e.matmul(
                        s_ps[ob][:], lhsT=ones_b[:], rhs=Pm[b3][:], start=(kt == 0), stop=(kt == n - 1)),
                        reads=[B_ones, BPm[b3]], writes=[BS[ob]] if kt == 0 else [], mark=True)

                prev = None
                for kt in range(n):
                    b3 = qk(kt)
                    if prev is not None:
                        pv(prev[0], prev[1])
                    prev = (kt, b3)
                pv(prev[0], prev[1])
                lastev = Ev(P.done["tensor"], P.done["tensor"].n)
                BO[ob].w = lastev
                BS[ob].w = lastev
                P.op("scalar", lambda e, ob=ob: e.activation(out=lnb[:], in_=s_ps[ob][:], func=AF.Ln),
                     reads=[BS[ob]], writes=[Bln])
                P.op("scalar", lambda e: e.activation(out=rinv[:], in_=lnb[:], func=AF.Exp, scale=-1.0),
                     reads=[Bln], writes=[Brinv])
                P.op("vector", lambda e, ob=ob, i=i, hh=hh: e.tensor_tensor(
                    out=mixT[:, 8 + 4 * hh:8 + 4 * hh + 4, i * 128:(i + 1) * 128],
                    in0=O_ps[ob][:].rearrange("p (h q) -> p h q", h=4),
                    in1=rinv[:].rearrange("p (h q) -> p h q", h=4), op=ALU.mult),
                    reads=[BO[ob], Brinv], writes=B_mix[8 + 4 * hh:8 + 4 * hh + 4])
        P.run_block()
        P.put_dma_sems(sems)
    mask_stack.close()
    if "stopD" in dbg:
        attn_stack.close(); top.close()
        return nc

    with ExitStack() as st:
        qd = [[sb("qd%d_%d" % (i, m), [128, TOWN], BF16, st) for m in range(2)] for i in range(2)]
        kd = [sb("kd%d" % i, [128, S], BF16, st) for i in range(2)]
        vd = [sb("vd%d" % i, [128, NT, 128], BF16, st) for i in range(2)]
        cmask = sb("cmask", [128, 32, 128], BF16, st)
        Eb = [sb("Ee%d" % i, [128, 2, 512], BF16, st) for i in range(3)]
        lamv = sb("lamv", [128, 4, 64], F32, st)
        lsm = sb("lsm", [128, 8], F32, st)
        lprod = sb("lprod", [128, 64], F32, st)
        gsub = sb("gsub", [128, 1], F32, st)
        f1 = sb("f1", [128, 512], F32, st)
        f2 = sb("f2", [128, 512], F32, st)
        r1 = sb("r1e", [128, 512], F32, st)
        r2 = sb("r2e", [128, 512], F32, st)
        dd = sb("dde", [128, 512], F32, st)
        o1s = sb("o1s", [128, 512], F32, st)
        o2s = sb("o2s", [128, 512], F32, st)
        sqb = sb("sqbe", [128, 512], BF16, st)
        rs = sb("rse", [128, 512], F32, st)
        sp_ps = [ps("sp_ps%d" % i, [128, 2, 512], F32, st) for i in range(2)]
        O1 = ps("O1", [128, 512], F32, st)
        O2 = ps("O2", [128, 512], F32, st)
        S1 = ps("S1", [128, 512], F32, st)
        S2 = ps("S2", [128, 512], F32, st)
        sems = P.get_dma_sems(12)
        Bcm, Blam, Blsm, Bgs = Buf(), [Buf() for _ in range(4)], Buf(), Buf()
        P.dma("gpsimd", sems[0], cmask[:], cst_maskT, writes=[Bcm])
        for n_, lv in enumerate([lam_q1, lam_k1, lam_q2, lam_k2]):
            P.dma("sync", sems[1 + n_], lamv[:, n_, :], lv.to_broadcast([128, 64]), writes=[Blam[n_]])
        P.dma("sync", sems[5], gsub[:], g_diff_sub.rearrange("o p -> p o"), writes=[Bgs])
        Blp = Buf()
        for m_ in range(2):
            P.op("vector", lambda e, m_=m_: e.tensor_tensor(out=lprod[:], in0=lamv[:, 2 * m_, :], in1=lamv[:, 2 * m_ + 1, :], op=ALU.mult),
                 reads=[Blam[2 * m_], Blam[2 * m_ + 1]], writes=[Blp])
            P.op("vector", lambda e, m_=m_: e.tensor_reduce(out=lsm[:, m_:m_ + 1], in_=lprod[:], axis=AX.X, op=ALU.add),
                 reads=[Blp], writes=[Blsm])
        P.op("scalar", lambda e: e.activation(out=lsm[:, 2:4], in_=lsm[:, 0:2], func=AF.Exp), reads=[Blsm], writes=[Blsm])
        P.op("vector", lambda e: e.scalar_tensor_tensor(out=lsm[:, 4:5], in0=lsm[:, 3:4], scalar=float(LAM_INIT), in1=lsm[:, 2:3],
                                                        op0=ALU.subtract, op1=ALU.subtract), reads=[Blsm], writes=[Blsm])
        P.op("vector", lambda e: e.tensor_scalar(out=lsm[:, 5:6], in0=gsub[:], scalar1=float(1.0 - LAM_INIT), scalar2=None, op0=ALU.mult),
             reads=[Bgs, Blsm], writes=[Blsm])
        Bqd, Bkd, Bvd = [Buf(), Buf()], [Buf(), Buf()], [Buf(), Buf()]
        BE, Bsp = [Buf() for _ in range(3)], [Buf(), Buf()]
        BO1, BO2, BS1, BS2 = Buf(), Buf(), Buf(), Buf()
        Bf1, Bf2, Br1, Br2, Bdd, Bsqb, Brs = [Buf() for _ in range(7)]
        Bo1s, Bo2s = Buf(), Buf()
        deferred = [None]
        SCD = 64 ** -0.5

        Bqz = Buf()
        for i_ in range(2):
            for m_ in range(2):
                P.op("vector", lambda e, i_=i_, m_=m_: e.memset(qd[i_][m_][:], 0.0), writes=[Bqz])

        def load_head(h):
            hb = h % 2
            P.dma("sync", sems[6 + hb], qd[hb][0][0:64, :], qdT_scr[h][0:64, :], reads=[Bqz], writes=[Bqd[hb]])
            P.dma("sync", sems[6 + hb], qd[hb][1][64:128, :], qdT_scr[h][64:128, :], reads=[Bqz], writes=[Bqd[hb]])
            P.dma("sync", sems[8 + hb], kd[hb][:], kdT_scr[h], writes=[Bkd[hb]])
            P.dma("gpsimd", sems[10 + hb], vd[hb][:], vd_scr[:, h * 128:(h + 1) * 128].rearrange("(t p) d -> p t d", p=128),
                  writes=[Bvd[hb]])

        load_head(0)
        ecnt = [0]
        for h in range(8):
            hb = h % 2
            if h + 1 < 8:
                load_head(h + 1)
            for qc in range(2):
                slots = list(range(4 * qc, 4 * qc + 4))
                nmax = NTL[slots[-1]]

                def c0_of(kt, slots=slots):
                    first = sum(1 for s_ in slots if NTL[s_] <= kt)
                    return 128 * first

                def qk(kt, hb=hb, qc=qc, slots=slots):
                    c0 = c0_of(kt)
                    sb2 = ecnt[0] % 2
                    eb = ecnt[0] % 3
                    ecnt[0] += 1
                    for m_ in range(2):
                        P.op("tensor", lambda e, m_=m_, sb2=sb2, kt=kt, c0=c0, hb=hb, qc=qc: e.matmul(
                            sp_ps[sb2][:, m_, c0:512], lhsT=kd[hb][:, kt * 128:(kt + 1) * 128],
                            rhs=qd[hb][m_][:, qc * 512 + c0:(qc + 1) * 512], start=True, stop=True),
                            reads=[Bkd[hb], Bqd[hb]], writes=[Bsp[sb2]] if m_ == 0 else [], mark=(m_ == 1))
                    P.op("scalar", lambda e, sb2=sb2, eb=eb, c0=c0: e.activation(
                        out=Eb[eb][:, :, c0:512], in_=sp_ps[sb2][:, :, c0:512], func=AF.Exp, scale=SCD),
                        reads=[Bsp[sb2]], writes=[BE[eb]])
                    for lc, s_ in enumerate(slots):
                        nn = NTL[s_]
                        if nn - 4 <= kt < nn:
                            jj = kt - (nn - 4)
                            P.op("vector", lambda e, eb=eb, lc=lc, s_=s_, jj=jj: e.tensor_tensor(
                                out=Eb[eb][:, :, lc * 128:(lc + 1) * 128], in0=Eb[eb][:, :, lc * 128:(lc + 1) * 128],
                                in1=cmask[:, s_ * 4 + jj, :].unsqueeze(1).to_broadcast([128, 2, 128]), op=ALU.mult),
                                reads=[BE[eb], Bcm], writes=[BE[eb]])
                    return (eb, c0)

                def pv(kt, eb, c0, hb=hb, nmax=nmax):
                    first = (kt == 0)
                    last = (kt == nmax - 1)
                    for m_, (Ot, St, BOt, BSt) in enumerate([(O1, S1, BO1, BS1), (O2, S2, BO2, BS2)]):
                        P.op("tensor", lambda e, m_=m_, Ot=Ot, eb=eb, c0=c0, kt=kt, hb=hb, first=first, last=last: e.matmul(
                            Ot[:, c0:512], lhsT=vd[hb][:, kt, :], rhs=Eb[eb][:, m_, c0:512], start=first, stop=last,
                            skip_group_check=True),
                            reads=[Bvd[hb], BE[eb]], writes=[BOt] if first else [], mark=False)
                        P.op("tensor", lambda e, m_=m_, St=St, eb=eb, c0=c0, first=first, last=last: e.matmul(
                            St[:, c0:512], lhsT=ones_b[:], rhs=Eb[eb][:, m_, c0:512], start=first, stop=last,
                            skip_group_check=True),
                            reads=[B_ones, BE[eb]], writes=[BSt] if first else [], mark=(m_ == 1))

                prev = None
                for kt in range(nmax):
                    cur = qk(kt)
                    if prev is not None:
                        pv(prev[0], prev[1], prev[2])
                    prev = (kt, cur[0], cur[1])
                    if kt == 4 and deferred[0] is not None:
                        deferred[0]()
                        deferred[0] = None
                pv(prev[0], prev[1], prev[2])
                lastev = Ev(P.done["tensor"], P.done["tensor"].n)
                for B_ in (BO1, BO2, BS1, BS2):
                    B_.w = lastev
                P.op("vector", lambda e: e.tensor_copy(out=o1s[:], in_=O1[:]), reads=[BO1], writes=[Bo1s])
                P.op("vector", lambda e: e.tensor_copy(out=o2s[:], in_=O2[:]), reads=[BO2], writes=[Bo2s])
                P.op("scalar", lambda e: e.activation(out=f1[:], in_=S1[:], func=AF.Ln), reads=[BS1], writes=[Bf1])
                P.op("scalar", lambda e: e.activation(out=f2[:], in_=S2[:], func=AF.Ln), reads=[BS2], writes=[Bf2])
                P.op("scalar", lambda e: e.activation(out=r1[:], in_=f1[:], func=AF.Exp, scale=-1.0), reads=[Bf1], writes=[Br1])
                P.op("scalar", lambda e: e.activation(out=r2[:], in_=f2[:], func=AF.Exp, scale=-1.0), reads=[Bf2], writes=[Br2])
                P.op("vector", lambda e: e.tensor_tensor(out=f1[:], in0=o1s[:], in1=r1[:], op=ALU.mult),
                     reads=[Bo1s, Br1], writes=[Bf1])
                P.op("vector", lambda e: e.tensor_tensor(out=f2[:], in0=o2s[:], in1=r2[:], op=ALU.mult),
                     reads=[Bo2s, Br2], writes=[Bf2])
                P.op("vector", lambda e: e.scalar_tensor_tensor(out=dd[:], in0=f2[:], scalar=lsm[:, 4:5], in1=f1[:],
                                                                op0=ALU.mult, op1=ALU.add),
                     reads=[Bf1, Bf2, Blsm], writes=[Bdd])
                P.op("scalar", lambda e: e.activation(out=sqb[:], in_=dd[:], func=AF.Square), reads=[Bdd], writes=[Bsqb])

                def part2(h=h, qc=qc):
                    msb = ecnt[0] % 2
                    ecnt[0] += 1
                    P.op("tensor", lambda e, msb=msb: e.matmul(sp_ps[msb][:, 0, :], lhsT=ones_b[:], rhs=sqb[:], start=True, stop=True),
                         reads=[Bsqb, B_ones], writes=[Bsp[msb]], mark=True)
                    P.op("scalar", lambda e, msb=msb: e.activation(out=rs[:], in_=sp_ps[msb][:, 0, :], func=AF.Sqrt,
                                                                   scale=1.0 / 128.0, bias=epsT[:]),
                         reads=[Bsp[msb], B_eps], writes=[Brs])
                    P.op("vector", lambda e: e.reciprocal(out=rs[:], in_=rs[:]), reads=[Brs], writes=[Brs])
                    P.op("vector", lambda e, h=h, qc=qc: e.scalar_tensor_tensor(
                        out=mixT[:, h, qc * 512:(qc + 1) * 512], in0=dd[:], scalar=lsm[:, 5:6], in1=rs[:],
                        op0=ALU.mult, op1=ALU.mult), reads=[Bdd, Brs, Blsm], writes=[B_mix[h]])

                deferred[0] = part2
        if deferred[0] is not None:
            deferred[0]()
            deferred[0] = None
        if "dbgE" in dbg:
            dsem = P.get_dma_sems(1)
            mixf = sb("mixf", [128, 4096], F32, st)
            Bmf = Buf()
            P.op("vector", lambda e: e.tensor_copy(out=mixf[:, 0:1024], in_=mixT[:, 0, :]), reads=B_mix, writes=[Bmf])
            P.op("vector", lambda e: e.tensor_copy(out=mixf[:, 1024:2048], in_=mixT[:, 5, :]), reads=B_mix, writes=[Bmf])
            P.op("vector", lambda e: e.tensor_copy(out=mixf[:, 2048:3072], in_=mixT[:, 8, :]), reads=B_mix, writes=[Bmf])
            P.op("vector", lambda e: e.tensor_copy(out=mixf[:, 3072:4096], in_=mixT[:, 15, :]), reads=B_mix, writes=[Bmf])
            P.dma("sync", dsem[0], dbg_scr[:, 0:4096], mixf[:], reads=[Bmf])
            P.run_block(final_waits=[Ev(dsem[0], dsem[0].n)])
        else:
            P.run_block()
        P.put_dma_sems(sems)
    if "stopE" in dbg:
        attn_stack.close(); top.close()
        return nc

    def row_gain(tag, chunk, gvec, stack, sems2):
        ga = sb("ga" + tag, [128, D], F32, stack)
        gt = sb("gt" + tag, [128, D], F32, stack)
        Bga, Bgt = Buf(), Buf()
        P.dma("sync", sems2[0], ga[:], mod_scr[0:1, chunk * D:(chunk + 1) * D].to_broadcast([128, D]), writes=[Bga])
        P.dma("sync", sems2[1], gt[:], gvec.to_broadcast([128, D]), writes=[Bgt])
        P.op("vector", lambda e: e.tensor_tensor(out=ga[:], in0=ga[:], in1=gt[:], op=ALU.mult), reads=[Bga, Bgt], writes=[Bga])
        return ga, Bga

    def norm_residual(tag, stack, src_tiles, Bsrc, ssq, Bssq, nparts, resid_src, ga, Bga, dst):
        xr = [sb("xr%d%s" % (i, tag), [128, D], F32, stack) for i in range(2)]
        og = [sb("og%d%s" % (i, tag), [128, D], F32, stack) for i in range(2)]
        s8 = sb("s8" + tag, [128, 8], F32, stack)
        Bs8 = [Buf() for _ in range(8)]
        xring = Ring(P, [Buf(), Buf()])
        oring = Ring(P, [Buf(), Buf()])
        for tt in range(8):
            xb, xsem = xring.next()
            ob, osem = oring.next()
            b2 = tt % 2
            P.dma("sync", xsem, xr[b2][:], resid_src[tt * 128:(tt + 1) * 128, :], writes=[xb])
            P.op("vector", lambda e, tt=tt: e.tensor_reduce(out=s8[:, tt:tt + 1], in_=ssq[:, tt * 4:tt * 4 + nparts], axis=AX.X, op=ALU.add),
                 reads=[Bssq[tt]], writes=[Bs8[tt]])
            P.op("scalar", lambda e, tt=tt: e.activation(out=s8[:, tt:tt + 1], in_=s8[:, tt:tt + 1], func=AF.Sqrt,
                                                         scale=1.0 / D, bias=epsT[:]),
                 reads=[Bs8[tt], B_eps], writes=[Bs8[tt]])
            P.op("vector", lambda e, tt=tt: e.reciprocal(out=s8[:, tt:tt + 1], in_=s8[:, tt:tt + 1]),
                 reads=[Bs8[tt]], writes=[Bs8[tt]])
            P.op("vector", lambda e, tt=tt, b2=b2: e.scalar_tensor_tensor(
                out=og[b2][:], in0=src_tiles(tt), scalar=s8[:, tt:tt + 1], in1=ga[:], op0=ALU.mult, op1=ALU.mult),
                reads=[Bsrc[tt], Bs8[tt], Bga], writes=[ob])
            P.op("vector", lambda e, b2=b2: e.tensor_tensor(out=og[b2][:], in0=og[b2][:], in1=xr[b2][:], op=ALU.add),
                 reads=[xb, ob], writes=[ob])
            P.dma("sync", osem, dst[tt * 128:(tt + 1) * 128, :], og[b2][:], reads=[ob])
        return [Ev(s_, s_.n) for s_ in oring.sems], [xring, oring]

    with ExitStack() as st:
        wob = [sb("wob%d" % i, [128, KC, 512], BF16, st) for i in range(2)]
        mixed = sb("mixed", [128, 8, D], F32, st)
        ssq = sb("ssqF", [128, 32], F32, st)
        junkf = sb("junkf", [128, 512], BF16, st)
        acc = [ps("accF%d" % i, [128, 512], F32, st) for i in range(4)]
        Bacc = [Buf() for _ in range(4)]
        Bmixed, Bssq = [Buf() for _ in range(8)], [Buf() for _ in range(8)]
        Bjk = Buf()
        wring = Ring(P, [Buf(), Buf()])
        sems2 = P.get_dma_sems(2)
        if "noRG" not in dbg:
            ga, Bga = row_gain("F", 2, g_attn_post, st, sems2)
        w_o_v = w_o.rearrange("(k p) n -> p k n", p=128)
        P.op("vector", lambda e: e.memset(ssq[:], 0.0), writes=Bssq)
        wbs = []
        for cb in range(2):
            wb, wsem = wring.next()
            P.dma("gpsimd", wsem, wob[cb % 2][:], w_o_v[:, :, cb * 512:(cb + 1) * 512], writes=[wb])
            wbs.append(wb)
        na = 0
        for cb in range(4):
            wb = wbs[cb]
            for tt in range(8):
                ab = na % 4
                na += 1
                for k in range(KC):
                    P.op("tensor", lambda e, ab=ab, k=k, tt=tt, cb=cb: e.matmul(
                        acc[ab][:], lhsT=mixT[:, k, tt * 128:(tt + 1) * 128], rhs=wob[cb % 2][:, k, :],
                        start=(k == 0), stop=(k == KC - 1)),
                        reads=[B_mix[k], wb], writes=[Bacc[ab]] if k == 0 else [], mark=(k == KC - 1))
                P.op("vector", lambda e, ab=ab, tt=tt, cb=cb: e.tensor_copy(out=mixed[:, tt, cb * 512:(cb + 1) * 512], in_=acc[ab][:]),
                     reads=[Bacc[ab]], writes=[Bmixed[tt]])
                P.op("scalar", lambda e, tt=tt, cb=cb: e.activation(
                    out=junkf[:], in_=mixed[:, tt, cb * 512:(cb + 1) * 512], func=AF.Square,
                    accum_out=ssq[:, tt * 4 + cb:tt * 4 + cb + 1]),
                    reads=[Bmixed[tt]], writes=[Bjk, Bssq[tt]])
            if cb + 2 < 4:
                wb2, wsem = wring.next()
                P.dma("gpsimd", wsem, wob[cb % 2][:], w_o_v[:, :, (cb + 2) * 512:(cb + 3) * 512], writes=[wb2])
                wbs.append(wb2)
        if "stopF1" in dbg:
            P.run_block()
            st.close(); attn_stack.close(); top.close()
            return nc
        tails, rings = norm_residual("F", st, lambda tt: mixed[:, tt, :], Bmixed, ssq, Bssq, 4, x_own, ga, Bga, x1_scr)
        P.run_block(final_waits=tails)
        for r_ in rings + [wring]:
            r_.release()
        P.put_dma_sems(sems2)
    attn_stack.close()
    if "stopF" in dbg:
        top.close()
        return nc

    ffn_stack = ExitStack()
    aT = sb("aT", [128, NFF, TOWN], BF16, ffn_stack)
    B_aT = [Buf() for _ in range(NFF)]
    with ExitStack() as st:
        h2T = sb("h2T", [128, KC, TOWN], BF16, st)
        Bh2 = [Buf() for _ in range(8)]
        with ExitStack() as st1:
            xt = [sb("xtg%d" % i, [128, D], F32, st1) for i in range(2)]
            xs = [sb("xsg%d" % i, [128, D], BF16, st1) for i in range(2)]
            junk = sb("junkg", [128, D], BF16, st1)
            ss = sb("ssg", [128, 8], F32, st1)
            tp = [ps("tpg%d" % i, [128, KC, 128], BF16, st1) for i in range(2)]
            xring = Ring(P, [Buf(), Buf()])
            Bxs, Btp, Bss, Bjk = [Buf(), Buf()], [Buf(), Buf()], [Buf() for _ in range(8)], Buf()
            P.op("vector", lambda e: e.memset(ss[:], 0.0), writes=Bss)
            for tt in range(8):
                xb, xsem = xring.next()
                b2 = tt % 2
                P.dma("sync", xsem, xt[b2][:], x1_scr[tt * 128:(tt + 1) * 128, :], writes=[xb])
                P.op("scalar", lambda e, tt=tt, b2=b2: e.activation(
                    out=junk[:], in_=xt[b2][:], func=AF.Square, accum_out=ss[:, tt:tt + 1]),
                    reads=[xb], writes=[Bjk, Bss[tt]])
                P.op("scalar", lambda e, tt=tt: e.activation(
                    out=ss[:, tt:tt + 1], in_=ss[:, tt:tt + 1], func=AF.Sqrt, scale=1.0 / D, bias=epsT[:]),
                    reads=[Bss[tt], B_eps], writes=[Bss[tt]])
                P.op("vector", lambda e, tt=tt: e.reciprocal(out=ss[:, tt:tt + 1], in_=ss[:, tt:tt + 1]),
                     reads=[Bss[tt]], writes=[Bss[tt]])
                P.op("vector", lambda e, tt=tt, b2=b2: e.tensor_scalar(
                    out=xs[b2][:], in0=xt[b2][:], scalar1=ss[:, tt:tt + 1], scalar2=None, op0=ALU.mult),
                    reads=[xb, Bss[tt]], writes=[Bxs[b2]])
                for k in range(KC):
                    P.op("tensor", lambda e, b2=b2, k=k: e.transpose(
                        out=tp[b2][:, k, :], in_=xs[b2][:, k * 128:(k + 1) * 128], identity=ident_b[:]),
                        reads=[Bxs[b2], B_idb], writes=[Btp[b2]] if k == 0 else [], mark=(k == KC - 1))
                for k in range(KC):
                    P.op("scalar", lambda e, b2=b2, k=k, tt=tt: e.activation(
                        out=h2T[:, k, tt * 128:(tt + 1) * 128], in_=tp[b2][:, k, :], func=AF.Identity,
                        scale=modA[:, 2, k:k + 1], bias=modA[:, 3, k:k + 1]),
                        reads=[Btp[b2], B_mod], writes=[Bh2[tt]] if k == 0 else [])
                Bh2[tt].w = Ev(P.done["scalar"], P.done["scalar"].n)
            P.run_block()
            xring.release()
        with ExitStack() as st2:
            wg = [sb("wg%d" % i, [128, KC, 256], BF16, st2) for i in range(2)]
            wu = [sb("wu%d" % i, [128, KC, 256], BF16, st2) for i in range(2)]
            sg = [sb("sg%d" % i, [128, 512], F32, st2) for i in range(2)]
            pg = [ps("pg%d" % i, [128, 512], F32, st2) for i in range(2)]
            pu = [ps("pu%d" % i, [128, 512], F32, st2) for i in range(2)]
            Bpg, Bpu, Bsg = [Buf(), Buf()], [Buf(), Buf()], [Buf(), Buf()]
            gring = Ring(P, [Buf(), Buf()])
            uring = Ring(P, [Buf(), Buf()])
            wgv = w_gate.rearrange("(k p) n -> p k n", p=128)
            wuv = w_up.rearrange("(k p) n -> p k n", p=128)
            NB = NFF // 2
            wl = {}

            def loadw(fb):
                gb, gsem = gring.next()
                ub, usem = uring.next()
                P.dma("gpsimd", gsem, wg[fb % 2][:], wgv[:, :, fb * 256:(fb + 1) * 256], writes=[gb])
                P.dma("gpsimd", usem, wu[fb % 2][:], wuv[:, :, fb * 256:(fb + 1) * 256], writes=[ub])
                wl[fb] = (gb, ub)

            loadw(0)
            loadw(1)
            nn_ = 0
            for fb in range(NB):
                gb, ub = wl[fb]
                for ft in range(2):
                    fi = fb * 2 + ft
                    for tc in range(2):
                        b2 = nn_ % 2
                        nn_ += 1
                        hb_ = Bh2[tc * 4:(tc + 1) * 4]
                        for k in range(KC):
                            P.op("tensor", lambda e, b2=b2, k=k, fb=fb, ft=ft, tc=tc: e.matmul(
                                pg[b2][:], lhsT=wg[fb % 2][:, k, ft * 128:(ft + 1) * 128], rhs=h2T[:, k, tc * 512:(tc + 1) * 512],
                                start=(k == 0), stop=(k == KC - 1)),
                                reads=[gb] + hb_, writes=[Bpg[b2]] if k == 0 else [], mark=(k == KC - 1))
                        for k in range(KC):
                            P.op("tensor", lambda e, b2=b2, k=k, fb=fb, ft=ft, tc=tc: e.matmul(
                                pu[b2][:], lhsT=wu[fb % 2][:, k, ft * 128:(ft + 1) * 128], rhs=h2T[:, k, tc * 512:(tc + 1) * 512],
                                start=(k == 0), stop=(k == KC - 1)),
                                reads=[ub] + hb_, writes=[Bpu[b2]] if k == 0 else [], mark=(k == KC - 1))
                        P.op("scalar", lambda e, b2=b2: e.activation(out=sg[b2][:], in_=pg[b2][:], func=AF.Silu),
                             reads=[Bpg[b2]], writes=[Bsg[b2]])
                        P.op("vector", lambda e, b2=b2, fi=fi, tc=tc: e.tensor_tensor(
                            out=aT[:, fi, tc * 512:(tc + 1) * 512], in0=sg[b2][:], in1=pu[b2][:], op=ALU.mult),
                            reads=[Bsg[b2], Bpu[b2]], writes=[B_aT[fi]])
                if fb + 2 < NB:
                    loadw(fb + 2)
            P.run_block()
            gring.release()
            uring.release()
    ssqG = sb("ssqG", [128, 32], F32, ffn_stack)
    BssqG = [Buf() for _ in range(8)]
    with ExitStack() as st:
        wd = [sb("wd%d" % i, [128, NFF, 512], BF16, st) for i in range(2)]
        fst = [sb("fst%d" % i, [128, 512], F32, st) for i in range(3)]
        junkd = sb("junkd", [128, 512], BF16, st)
        acc = [ps("accG%d" % i, [128, 512], F32, st) for i in range(4)]
        Bacc = [Buf() for _ in range(4)]
        Bjk = Buf()
        wring = Ring(P, [Buf(), Buf()])
        fring = Ring(P, [Buf(), Buf(), Buf()])
        wdv = w_down.rearrange("(k p) n -> p k n", p=128)
        P.op("vector", lambda e: e.memset(ssqG[:], 0.0), writes=BssqG)
        wbs = []
        for cb in range(2):
            wb, wsem = wring.next()
            P.dma("gpsimd", wsem, wd[cb % 2][:, 0:22, :], wdv[:, 0:22, cb * 512:(cb + 1) * 512], writes=[wb])
            P.dma("gpsimd", wsem, wd[cb % 2][:, 22:44, :], wdv[:, 22:44, cb * 512:(cb + 1) * 512], writes=[wb])
            wbs.append(wb)
        na = 0
        for cb in range(4):
            wb = wbs[cb]
            for tt in range(8):
                ab = na % 4
                na += 1
                for k in range(NFF):
                    P.op("tensor", lambda e, ab=ab, k=k, tt=tt, cb=cb: e.matmul(
                        acc[ab][:], lhsT=aT[:, k, tt * 128:(tt + 1) * 128], rhs=wd[cb % 2][:, k, :],
                        start=(k == 0), stop=(k == NFF - 1)),
                        reads=[B_aT[k], wb], writes=[Bacc[ab]] if k == 0 else [], mark=(k == NFF - 1))
                fb_, fsem = fring.next()
                fi_ = (fring.k - 1) % 3
                P.op("vector", lambda e, ab=ab, fi_=fi_: e.tensor_copy(out=fst[fi_][:], in_=acc[ab][:]),
                     reads=[Bacc[ab]], writes=[fb_])
                P.op("scalar", lambda e, fi_=fi_, tt=tt, cb=cb: e.activation(
                    out=junkd[:], in_=fst[fi_][:], func=AF.Square, accum_out=ssqG[:, tt * 4 + cb:tt * 4 + cb + 1]),
                    reads=[fb_], writes=[Bjk, BssqG[tt]])
                P.dma("sync", fsem, ff_scr[tt * 128:(tt + 1) * 128, cb * 512:(cb + 1) * 512], fst[fi_][:], reads=[fb_])
            if cb + 2 < 4:
                wb2, wsem = wring.next()
                P.dma("gpsimd", wsem, wd[cb % 2][:, 0:22, :], wdv[:, 0:22, (cb + 2) * 512:(cb + 3) * 512], writes=[wb2])
                P.dma("gpsimd", wsem, wd[cb % 2][:, 22:44, :], wdv[:, 22:44, (cb + 2) * 512:(cb + 3) * 512], writes=[wb2])
                wbs.append(wb2)
        P.run_block(final_waits=[Ev(s_, s_.n) for s_ in fring.sems])
        wring.release()
        fring.release()
    with ExitStack() as st:
        fft = [sb("fft%d" % i, [128, D], F32, st) for i in range(2)]
        sems2 = P.get_dma_sems(2)
        ga, Bga = row_gain("G", 5, g_ffn_post, st, sems2)
        ffring = Ring(P, [Buf(), Buf()])
        Bff = []
        xr = None
        srcB = [None] * 8

        def src_tiles(tt):
            return fft[tt % 2][:]

        xrr = [sb("xrG%d" % i, [128, D], F32, st) for i in range(2)]
        og = [sb("ogG%d" % i, [128, D], F32, st) for i in range(2)]
        s8 = sb("s8G", [128, 8], F32, st)
        Bs8 = [Buf() for _ in range(8)]
        xring = Ring(P, [Buf(), Buf()])
        oring = Ring(P, [Buf(), Buf()])
        for tt in range(8):
            fb_, fsem = ffring.next()
            xb, xsem = xring.next()
            ob, osem = oring.next()
            b2 = tt % 2
            P.dma("sync", fsem, fft[b2][:], ff_scr[tt * 128:(tt + 1) * 128, :], writes=[fb_])
            P.dma("sync", xsem, xrr[b2][:], x1_scr[tt * 128:(tt + 1) * 128, :], writes=[xb])
            P.op("vector", lambda e, tt=tt: e.tensor_reduce(out=s8[:, tt:tt + 1], in_=ssqG[:, tt * 4:tt * 4 + 4], axis=AX.X, op=ALU.add),
                 reads=[BssqG[tt]], writes=[Bs8[tt]])
            P.op("scalar", lambda e, tt=tt: e.activation(out=s8[:, tt:tt + 1], in_=s8[:, tt:tt + 1], func=AF.Sqrt,
                                                         scale=1.0 / D, bias=epsT[:]),
                 reads=[Bs8[tt], B_eps], writes=[Bs8[tt]])
            P.op("vector", lambda e, tt=tt: e.reciprocal(out=s8[:, tt:tt + 1], in_=s8[:, tt:tt + 1]),
                 reads=[Bs8[tt]], writes=[Bs8[tt]])
            P.op("vector", lambda e, tt=tt, b2=b2: e.scalar_tensor_tensor(
                out=og[b2][:], in0=fft[b2][:], scalar=s8[:, tt:tt + 1], in1=ga[:], op0=ALU.mult, op1=ALU.mult),
                reads=[fb_, Bs8[tt], Bga], writes=[ob])
            P.op("vector", lambda e, b2=b2: e.tensor_tensor(out=og[b2][:], in0=og[b2][:], in1=xrr[b2][:], op=ALU.add),
                 reads=[xb, ob], writes=[ob])
            P.dma("sync", osem, out_own[tt * 128:(tt + 1) * 128, :], og[b2][:], reads=[ob])
        P.run_block(final_waits=[Ev(s_, s_.n) for s_ in oring.sems])
    ffn_stack.close()
    top.close()
    return nc


def host_constants(r):
    cst = np.zeros((128, 8), np.float32)
    for p in range(128):
        d = p % 64
        if d < 16:
            cst[p, 0] = np.float32(THETA) ** np.float32(-((2 * (d % 8)) / 16.0))
        if p < 32:
            cst[p, 1] = np.float32(THETA) ** np.float32(-((2 * (p % 16)) / 32.0))
    rm = np.zeros((2, 128, 128), np.float32)
    for m in range(128):
        d = m % 64
        base = m - d
        if d < 8:
            rm[0, base + d + 8, m] = -1.0
        elif d < 16:
            rm[0, base + d - 8, m] = 1.0
        if m < 16:
            rm[1, m + 16, m] = -1.0
        elif m < 32:
            rm[1, m - 16, m] = 1.0
    ident = np.eye(128, dtype=np.float32)
    dmask = np.zeros((128, 32), np.float32)
    for p in range(128):
        dmask[p, p % 32] = 1.0
    bis = np.zeros((128, 2 * NBIS), np.float32)
    for it in range(NBIS):
        bis[:, it] = 2.0 ** (-(it + 1))
        bis[:, NBIS + it] = 2.0 ** (-(it + 2)) if it < NBIS - 1 else 2.0 ** (-(it + 1))
    blocks = slot_blocks(r)
    maskT = np.zeros((128, 32, 128), np.float32)
    maskadd = np.zeros((128, 8, 512), np.float32)
    kk = np.arange(128)[:, None]
    qq = np.arange(128)[None, :]
    for i in range(8):
        n = slot_ntiles(i)
        for j in range(4):
            kt = n - 4 + j
            if kt < blocks[i]:
                m = np.ones((128, 128), np.float32)
            elif kt == blocks[i]:
                m = (kk <= qq).astype(np.float32)
            else:
                m = np.zeros((128, 128), np.float32)
            maskT[:, i * 4 + j, :] = m
            maskadd[:, i, j * 128:(j + 1) * 128] = (m.T - 1.0) * 1.0e30
    return dict(cst_f32=cst, cst_rm=rm, cst_ident=ident, cst_dmask=dmask, cst_bis=bis,
                cst_maskT=maskT, cst_maskadd=maskadd)


def make_in_maps(inputs):
    x = np.asarray(inputs["x"], np.float32)
    c = np.asarray(inputs["c"], np.float32)
    pos = np.asarray(inputs["positions"], np.int32)
    shared = {}
    for k in ["w_ada", "b_ada", "g_attn_pre", "g_attn_post", "g_ffn_pre", "g_ffn_post", "w_in",
              "lambda_q1", "lambda_k1", "lambda_q2", "lambda_k2", "g_diff_sub", "w_o", "w_gate", "w_up", "w_down"]:
        a = np.asarray(inputs[k], np.float32)[0]
        if a.ndim == 1:
            a = a[None, :]
        shared[k] = np.ascontiguousarray(a)
    in_maps = []
    for core in range(8):
        b, r = divmod(core, 4)
        blocks = slot_blocks(r)
        idx = np.concatenate([np.arange(bl * 128, (bl + 1) * 128) for bl in blocks])
        m = dict(shared)
        m["x_all"] = np.ascontiguousarray(x[b])
        m["x_own"] = np.ascontiguousarray(x[b][idx])
        m["pos_all"] = np.ascontiguousarray(pos[b][None, :])
        m["pos_own"] = np.ascontiguousarray(pos[b][idx][None, :])
        m["c_in"] = np.ascontiguousarray(c[b][None, :])
        m.update(host_constants(r))
        in_maps.append(m)
    return in_maps


def kernel(**inputs):
    nc = build_program()
    in_maps = make_in_maps(inputs)
    res = run_bass_kernel_spmd(nc, in_maps, core_ids=list(range(8)))
    out = np.zeros((2, S, D), np.float32)
    for core in range(8):
        b, r = divmod(core, 4)
        blocks = slot_blocks(r)
        o = res.results[core]["out_own"]
        for i, bl in enumerate(blocks):
            out[b, bl * 128:(bl + 1) * 128] = o[i * 128:(i + 1) * 128]
    return out
```

```python
import math
from contextlib import ExitStack

import numpy as np
import ml_dtypes
import concourse.bass as bass
import concourse.mybir as mybir
from concourse.bass_utils import run_bass_kernel_spmd

F32 = mybir.dt.float32
BF16 = mybir.dt.bfloat16
I32 = mybir.dt.int32
ALU = mybir.AluOpType
AF = mybir.ActivationFunctionType
AX = mybir.AxisListType

D = 2048
S = 4096
NT = 32
TOWN = 1024
DFF = 5632
NFF = DFF // 128
KC = 16
PROJ = 5456
O_DQ, O_DK, O_DV, O_SQ, O_SK, O_SV, O_IQ, O_IK, O_IW = 0, 1024, 2048, 3072, 4096, 4224, 4352, 5376, 5440
EPS = 1e-6
THETA = 500000.0
IDX_W_SCALE = (16 ** -0.5) * (64 ** -0.5)
LAM_INIT = 0.8 - 0.6 * math.exp(0.0)
NEG = -1.0e30
NBIS = 12
PI = math.pi

ENGS = ["sync", "scalar", "gpsimd", "vector", "tensor"]


def slot_blocks(r):
    out = []
    for g in range(4):
        out += [8 * g + r, 8 * g + 7 - r]
    return out


def slot_ntiles(i):
    g, hi = divmod(i, 2)
    return 8 * g + (8 if hi else 4)


class Sem:
    def __init__(self, nc, name):
        self.h = nc.alloc_semaphore(name=name)
        self.n = 0


_CUR_PHASE = [0]


class Ev:
    __slots__ = ("sem", "val", "phase")

    def __init__(self, sem=None, val=None, phase=None):
        self.sem = sem
        self.val = val
        self.phase = _CUR_PHASE[0] if phase is None else phase


class Buf:
    def __init__(self, ap=None):
        self.ap = ap
        self.w = None
        self.r = []


class Prog:
    def __init__(self, nc):
        self.nc = nc
        self.q = {e: [] for e in ENGS}
        self.done = {e: Sem(nc, "done_" + e) for e in ["scalar", "gpsimd", "vector", "tensor"]}
        self.pending = {e: [] for e in ENGS}
        self.dma_pool = [Sem(nc, "dma%d" % i) for i in range(56)]
        self.dma_free = list(self.dma_pool)
        self.all_pending = []
        self.phase = 0
        _CUR_PHASE[0] = 0
        self.phase_dma = {}
        self.twins = {}

    def get_dma_sems(self, n):
        out = [self.dma_free.pop() for _ in range(n)]
        return out

    def put_dma_sems(self, sems):
        self.dma_free.extend(sems)

    def _emit_wait(self, eng, ev):
        cur = self.phase

        def f(e, st, ev=ev):
            if ev.phase is not None and ev.phase < cur:
                return
            if ev.val is None:
                raise RuntimeError("wait on unresolved event (missing mark) on engine " + eng)
            if ev.val <= 0:
                return
            key = id(ev.sem)
            if st.get(key, 0) >= ev.val:
                return
            st[key] = ev.val
            e.wait_ge(ev.sem.h, ev.val)
        self.q[eng].append(f)

    def _deps(self, eng, reads, writes, extra):
        evs = []
        for b in reads:
            if b.w is not None:
                evs.append(b.w)
        for b in writes:
            if b.w is not None:
                evs.append(b.w)
            evs.extend(b.r)
        evs.extend(extra)
        seen = set()
        for ev in evs:
            if id(ev) in seen:
                continue
            seen.add(id(ev))
            self._emit_wait(eng, ev)

    def op(self, eng, fn, reads=(), writes=(), waits=(), mark=True):
        self._deps(eng, reads, writes, waits)
        if mark:
            sem = self.done[eng]
            sem.n += 1
            ev = Ev(sem, sem.n, self.phase)
            self.q[eng].append(lambda e, st, fn=fn, sem=sem: fn(e).then_inc(sem.h, 1))
            for pev in self.pending[eng]:
                pev.sem, pev.val = sem, sem.n
            self.pending[eng] = []
        else:
            ev = Ev(None, None, self.phase)
            self.pending[eng].append(ev)
            self.all_pending.append(ev)
            self.q[eng].append(lambda e, st, fn=fn: fn(e))
        for b in reads:
            b.r.append(ev)
        for b in writes:
            b.w = ev
            b.r = []
        return ev

    def dma(self, eng, sem, out, in_, reads=(), writes=(), waits=(), **kw):
        if eng == "gpsimd":
            tw = self.twins.get(id(sem))
            if tw is None:
                tw = Sem(self.nc, "sw%d" % len(self.twins))
                self.twins[id(sem)] = tw
            sem = tw
        if eng == "gpsimd":
            hist = getattr(self, "_gp_hist", [])
            if len(hist) >= 3:
                self._emit_wait(eng, hist[-3])
            self._gp_hist = hist
        self._deps(eng, reads, writes, waits)
        sem.n += 16
        ev = Ev(sem, sem.n, self.phase)
        self.phase_dma[id(sem)] = ev
        self.q[eng].append(
            lambda e, st, out=out, in_=in_, sem=sem, kw=kw: e.dma_start(out=out, in_=in_, **kw).then_inc(sem.h, 16))
        if eng == "gpsimd":
            self._gp_hist.append(ev)
            self._gp_hist = self._gp_hist[-4:]
        for b in reads:
            b.r.append(ev)
        for b in writes:
            b.w = ev
            b.r = []
        return ev

    def wait(self, eng, ev):
        self._emit_wait(eng, ev)

    def run_block(self, final_waits=()):
        for ev in final_waits:
            self._emit_wait("sync", ev)
        for ev in self.phase_dma.values():
            self._emit_wait("sync", ev)
        self.phase_dma = {}
        with self.nc.allow_non_contiguous_dma(reason="small strided vector/table layouts"), self.nc.Block() as blk:
            for name in ENGS:
                ops = self.q[name]
                if not ops:
                    continue

                def body(e, ops=ops):
                    st = {}
                    for f in ops:
                        f(e, st)
                getattr(blk, name)(body)
        self.q = {e: [] for e in ENGS}
        for ev in self.all_pending:
            if ev.val is None:
                ev.val = 0
        self.all_pending = []
        self.pending = {e: [] for e in ENGS}
        self.phase += 1
        _CUR_PHASE[0] = self.phase


class Ring:
    def __init__(self, P, bufs, with_sems=True):
        self.bufs = bufs
        self.n = len(bufs)
        self.sems = P.get_dma_sems(self.n) if with_sems else None
        self.k = 0
        self.P = P

    def next(self):
        i = self.k % self.n
        self.k += 1
        return self.bufs[i], (self.sems[i] if self.sems else None)

    def release(self):
        if self.sems:
            self.P.put_dma_sems(self.sems)


def build_program(debug=None):
    nc = bass.Bass("TRN2", target_bir_lowering=False)
    dbg = debug or set()

    def din(name, shape, dt=F32):
        if ("tiny" in dbg and name in ("w_ada", "w_o", "w_gate", "w_up", "w_down")) or ("tiny:" + name) in dbg:
            nc.dram_tensor(name, [1, 1], dt, kind="ExternalInput")
            return None
        return nc.dram_tensor(name, list(shape), dt, kind="ExternalInput").ap()

    def dscr(name, shape, dt):
        kind = "ExternalOutput" if (name in dbg) else "Internal"
        return nc.dram_tensor(name, list(shape), dt, kind=kind).ap()

    x_all = din("x_all", [S, D])
    x_own = din("x_own", [TOWN, D])
    pos_all = din("pos_all", [1, S], I32)
    pos_own = din("pos_own", [1, TOWN], I32)
    c_in = din("c_in", [1, D])
    w_ada = din("w_ada", [D, 6 * D])
    b_ada = din("b_ada", [1, 6 * D])
    g_attn_pre = din("g_attn_pre", [1, D])
    g_attn_post = din("g_attn_post", [1, D])
    g_ffn_pre = din("g_ffn_pre", [1, D])
    g_ffn_post = din("g_ffn_post", [1, D])
    w_in = din("w_in", [D, PROJ])
    lam_q1 = din("lambda_q1", [1, 64])
    lam_k1 = din("lambda_k1", [1, 64])
    lam_q2 = din("lambda_q2", [1, 64])
    lam_k2 = din("lambda_k2", [1, 64])
    g_diff_sub = din("g_diff_sub", [1, 128])
    w_o = din("w_o", [D, D])
    w_gate = din("w_gate", [D, DFF])
    w_up = din("w_up", [D, DFF])
    w_down = din("w_down", [DFF, D])
    cst_f32 = din("cst_f32", [128, 8])
    cst_rm = din("cst_rm", [2, 128, 128])
    cst_ident = din("cst_ident", [128, 128])
    cst_dmask = din("cst_dmask", [128, 32])
    cst_bis = din("cst_bis", [128, 2 * NBIS])
    cst_maskT = din("cst_maskT", [128, 32, 128])
    cst_maskadd = din("cst_maskadd", [128, 8, 512])

    out_own = nc.dram_tensor("out_own", [TOWN, D], F32, kind="ExternalOutput").ap()

    mod_scr = dscr("mod_scr", [1, 6 * D], F32)
    kdT_scr = dscr("kdT_scr", [8, 128, S], BF16)
    vd_scr = dscr("vd_scr", [S, 1024], BF16)
    skT_scr = dscr("skT_scr", [128, S], BF16)
    sv_scr = dscr("sv_scr", [S, 128], BF16)
    ikT_scr = dscr("ikT_scr", [64, S], BF16)
    qdT_scr = dscr("qdT_scr", [8, 128, TOWN], BF16)
    sqT_scr = dscr("sqT_scr", [8, 128, TOWN], BF16)
    iqT_scr = dscr("iqT_scr", [8, 128, TOWN], BF16)
    x1_scr = dscr("x1_scr", [TOWN, D], F32)
    ff_scr = dscr("ff_scr", [TOWN, D], F32)
    dbg_scr = dscr("dbg_scr", [128, 4096], F32)

    P = Prog(nc)
    top = ExitStack()

    def sb(name, shape, dt, stack=top):
        return stack.enter_context(nc.sbuf_tensor(name, list(shape), dt))

    def ps(name, shape, dt, stack):
        return stack.enter_context(nc.psum_tensor(name, list(shape), dt))

    cstf = sb("cstf", [128, 8], F32)
    rm_f = sb("rm_f", [128, 2, 128], F32)
    rm_b = sb("rm_b", [128, 2, 128], BF16)
    ident_f = sb("ident_f", [128, 128], F32)
    ident_b = sb("ident_b", [128, 128], BF16)
    ones_b = sb("ones_b", [128, 128], BF16)
    negpi = sb("negpi", [128, 1], F32)
    epsT = sb("epsT", [128, 1], F32)
    halfpi = sb("halfpi", [128, 1], F32)
    B_eps = Buf()
    modA = sb("modA", [128, 4, KC], F32)
    iw_sb = sb("iw_sb", [128, 8, 16], F32)
    B_cstf, B_rmf, B_idf, B_rmb, B_idb, B_ones, B_negpi, B_mod, B_iw = [Buf() for _ in range(9)]

    s0 = P.get_dma_sems(3)
    P.dma("sync", s0[0], cstf[:], cst_f32, writes=[B_cstf])
    P.dma("sync", s0[1], rm_f[:], cst_rm.rearrange("k p m -> p k m"), writes=[B_rmf])
    P.dma("sync", s0[2], ident_f[:], cst_ident, writes=[B_idf])
    P.op("vector", lambda e: e.tensor_copy(out=rm_b[:], in_=rm_f[:]), reads=[B_rmf], writes=[B_rmb])
    P.op("vector", lambda e: e.tensor_copy(out=ident_b[:], in_=ident_f[:]), reads=[B_idf], writes=[B_idb])
    P.op("vector", lambda e: e.memset(ones_b[:], 1.0), writes=[B_ones])
    P.op("vector", lambda e: e.memset(negpi[:], -PI), writes=[B_negpi])
    P.op("vector", lambda e: e.memset(epsT[:], EPS), writes=[B_eps])
    P.op("vector", lambda e: e.memset(halfpi[:], 0.5 * PI), writes=[B_negpi])
    P.run_block()
    P.put_dma_sems(s0)

    if "skipA" in dbg:
        P.op("vector", lambda e: e.memset(modA[:], 0.0), writes=[B_mod])
        P.op("vector", lambda e: e.memset(modA[:, 0, :], 1.0), writes=[B_mod])
        P.op("vector", lambda e: e.memset(modA[:, 2, :], 1.0), writes=[B_mod])
        P.run_block()
    with ExitStack() as st:
      if "skipA" not in dbg:
          cT = sb("cT", [128, KC], F32, st)
          sc = sb("sc", [128, KC], BF16, st)
          brow = sb("brow", [1, 6 * D], F32, st)
          wab = [sb("wab%d" % i, [128, KC, 512], BF16, st) for i in range(3)]
          psa = [ps("psa%d" % i, [128, 512], F32, st) for i in range(2)]
          gv = sb("gv", [128, 2, KC], F32, st)
          mv = sb("mv", [128, 4, KC], F32, st)
          sems = P.get_dma_sems(8)
          Bc, Bsc, Bbrow, Bms = Buf(), Buf(), Buf(), Buf()
          wring = Ring(P, [Buf() for _ in range(3)])
          pring = [Buf(), Buf()]
          crow = sb("crow", [16, 128], F32, st)
          mrow = sb("mrow", [96, 128], F32, st)
          grow = sb("grow", [32, 128], F32, st)
          mvall = sb("mvall", [128, 96], F32, st)
          pst = ps("pst", [128, 128], F32, st)
          Bpst, Bmrow, Bgrow, Bmvall = Buf(), Buf(), Buf(), Buf()
          P.dma("sync", sems[0], crow[:], c_in.rearrange("o (k p) -> (o k) p", p=128), writes=[Bc])
          P.dma("sync", sems[1], brow[:], b_ada, writes=[Bbrow])
          P.op("tensor", lambda e: e.transpose(out=pst[:, 0:16], in_=crow[:], identity=ident_f[0:16, 0:16]),
               reads=[Bc, B_idf], writes=[Bpst], mark=True)
          P.op("scalar", lambda e: e.activation(out=sc[:], in_=pst[:, 0:16], func=AF.Silu), reads=[Bpst], writes=[Bsc])
          w_ada_v = w_ada.rearrange("(k p) n -> p k n", p=128)
          for j in range(24):
              wb, wsem = wring.next()
              i = (j % 3)
              P.dma("gpsimd", wsem, wab[i][:], w_ada_v[:, :, j * 512:(j + 1) * 512], writes=[wb])
              pb = pring[j % 2]
              for k in range(KC):
                  P.op("tensor",
                       lambda e, i=i, k=k, j=j: e.matmul(psa[j % 2][0:1, :], lhsT=sc[:, k:k + 1], rhs=wab[i][:, k, :],
                                                         start=(k == 0), stop=(k == KC - 1)),
                       reads=[Bsc, wb], writes=[pb] if k == 0 else [], mark=(k == KC - 1))
              P.op("vector",
                   lambda e, j=j: e.tensor_tensor(out=brow[0:1, j * 512:(j + 1) * 512], in0=psa[j % 2][0:1, :],
                                                  in1=brow[0:1, j * 512:(j + 1) * 512], op=ALU.add),
                   reads=[pb], writes=[Bbrow])
          P.dma("sync", sems[2], mod_scr, brow[:], reads=[Bbrow], writes=[Bms])
          P.dma("sync", sems[3], mrow[:], mod_scr.rearrange("o (r p) -> (o r) p", p=128), reads=[Bms], writes=[Bmrow])
          P.dma("sync", sems[4], grow[0:16, :], g_attn_pre.rearrange("o (k p) -> (o k) p", p=128), writes=[Bgrow])
          P.dma("sync", sems[5], grow[16:32, :], g_ffn_pre.rearrange("o (k p) -> (o k) p", p=128), writes=[Bgrow])
          gev = [Ev(sems[4], sems[4].n), Ev(sems[5], sems[5].n)]
          P.op("tensor", lambda e: e.transpose(out=pst[:, 0:96], in_=mrow[:], identity=ident_f[0:96, 0:96]),
               reads=[Bmrow, B_idf], writes=[Bpst], mark=True)
          P.op("vector", lambda e: e.tensor_copy(out=mvall[:], in_=pst[:, 0:96]), reads=[Bpst], writes=[Bmvall])
          P.op("tensor", lambda e: e.transpose(out=pst[:, 0:32], in_=grow[:], identity=ident_f[0:32, 0:32]),
               reads=[Bgrow, B_idf], writes=[Bpst], waits=gev, mark=True)
          Bgv2 = Buf()
          P.op("vector", lambda e: e.tensor_copy(out=gv[:].rearrange("p a k -> p (a k)"), in_=pst[:, 0:32]), reads=[Bpst], writes=[Bgv2])
          P.op("vector", lambda e: e.scalar_tensor_tensor(out=modA[:, 0, :], in0=mvall[:, 16:32], scalar=1.0, in1=gv[:, 0, :],
                                                          op0=ALU.add, op1=ALU.mult),
               reads=[Bmvall, Bgv2], writes=[B_mod])
          P.op("vector", lambda e: e.tensor_copy(out=modA[:, 1, :], in_=mvall[:, 0:16]), reads=[Bmvall], writes=[B_mod])
          P.op("vector", lambda e: e.scalar_tensor_tensor(out=modA[:, 2, :], in0=mvall[:, 64:80], scalar=1.0, in1=gv[:, 1, :],
                                                          op0=ALU.add, op1=ALU.mult),
               reads=[Bmvall, Bgv2], writes=[B_mod])
          P.op("vector", lambda e: e.tensor_copy(out=modA[:, 3, :], in_=mvall[:, 48:64]), reads=[Bmvall], writes=[B_mod])
          P.run_block()
          wring.release()
          P.put_dma_sems(sems)

    if "stopA" in dbg:
        top.close()
        return nc

    w_in_v = w_in.rearrange("(k p) n -> p k n", p=128)

    def project_set(tag, x_src, pos_src, wblocks):
        T = 1024
        with ExitStack() as st:
            hT = sb("hT" + tag, [128, KC, T], BF16, st)
            tabs = [sb("tab%d%s" % (i, tag), [128, T], F32, st) for i in range(4)]
            xt = [sb("xt%d%s" % (i, tag), [128, D], F32, st) for i in range(2)]
            xs = [sb("xs%d%s" % (i, tag), [128, D], BF16, st) for i in range(2)]
            junk = sb("junk" + tag, [128, D], BF16, st)
            ss = sb("ss" + tag, [128, 8], F32, st)
            rstd = sb("rstd" + tag, [128, 8], F32, st)
            wbuf = [sb("wb%d%s" % (i, tag), [128, KC, 512], BF16, st) for i in range(2)]
            pbs = [sb("pb%d%s" % (i, tag), [128, 512], BF16, st) for i in range(2)]
            t1s = [sb("t1%d%s" % (i, tag), [128, 512], BF16, st) for i in range(2)]
            t2s = [sb("t2%d%s" % (i, tag), [128, 512], BF16, st) for i in range(2)]
            stg = [sb("stg%d%s" % (i, tag), [128, 512], BF16, st) for i in range(3)]
            tp = [ps("tp%d%s" % (i, tag), [128, KC, 128], BF16, st) for i in range(2)]
            p1 = [ps("p1%d%s" % (i, tag), [128, 512], F32, st) for i in range(2)]
            p2 = [ps("p2%d%s" % (i, tag), [128, 512], F32, st) for i in range(2)]
            BhT = [Buf() for _ in range(8)]
            Btab = [Buf() for _ in range(4)]
            Bss, Brstd = [Buf() for _ in range(8)], [Buf() for _ in range(8)]
            Bjunk = Buf()
            xring = Ring(P, [Buf(), Buf()])
            Bxs = [Buf(), Buf()]
            Btp = [Buf(), Buf()]
            wring = Ring(P, [Buf(), Buf()])
            Bp1, Bp2, Bpb, Bt1, Bt2 = ([Buf(), Buf()] for _ in range(5))
            sring = Ring(P, [Buf(), Buf(), Buf()])
            psem = P.get_dma_sems(1)

            if True:
                posi = sb("posi" + tag, [128, T], I32, st)
                posf = sb("posf" + tag, [128, T], F32, st)
                ang = sb("ang" + tag, [128, T], F32, st)
                kf = sb("kf" + tag, [128, T], F32, st)
                Bposi, Bposf, Bang, Bkf = Buf(), Buf(), Buf(), Buf()
                P.dma("sync", psem[0], posi[:], pos_src.to_broadcast([128, T]), writes=[Bposi])
                P.op("vector", lambda e: e.tensor_copy(out=posf[:], in_=posi[:]), reads=[Bposi], writes=[Bposf])
                C1 = 6.28125
                C2 = float(np.float32(2.0 * PI - 6.28125))
                PIC = 3.1415925
                for kd in range(2):
                    P.op("vector", lambda e, kd=kd: e.tensor_scalar(
                        out=ang[:], in0=posf[:], scalar1=cstf[:, kd:kd + 1], scalar2=None, op0=ALU.mult),
                        reads=[Bposf, B_cstf], writes=[Bang])
                    P.op("vector", lambda e: e.tensor_scalar(
                        out=kf[:], in0=ang[:], scalar1=1.0 / (2.0 * PI), scalar2=None, op0=ALU.mult),
                        reads=[Bang], writes=[Bkf])
                    P.op("vector", lambda e: e.tensor_copy(out=posi[:], in_=kf[:]), reads=[Bkf, Bposf], writes=[Bposi])
                    P.op("vector", lambda e: e.tensor_copy(out=kf[:], in_=posi[:]), reads=[Bposi], writes=[Bkf])
                    P.op("vector", lambda e: e.scalar_tensor_tensor(
                        out=ang[:], in0=kf[:], scalar=-C1, in1=ang[:], op0=ALU.mult, op1=ALU.add),
                        reads=[Bkf, Bang], writes=[Bang])
                    P.op("vector", lambda e: e.scalar_tensor_tensor(
                        out=ang[:], in0=kf[:], scalar=-C2, in1=ang[:], op0=ALU.mult, op1=ALU.add),
                        reads=[Bkf, Bang], writes=[Bang])
                    P.op("vector", lambda e: e.tensor_scalar(
                        out=ang[:], in0=ang[:], scalar1=PIC, scalar2=-PIC, op0=ALU.min, op1=ALU.max),
                        reads=[Bang], writes=[Bang])
                    P.op("scalar", lambda e, kd=kd: e.activation(out=tabs[kd * 2 + 1][:], in_=ang[:], func=AF.Sin),
                         reads=[Bang], writes=[Btab[kd * 2 + 1]])
                    P.op("vector", lambda e: e.scalar_tensor_tensor(
                        out=kf[:], in0=ang[:], scalar=-1.0, in1=ang[:], op0=ALU.mult, op1=ALU.min),
                        reads=[Bang], writes=[Bkf])
                    P.op("scalar", lambda e, kd=kd: e.activation(out=tabs[kd * 2][:], in_=kf[:], func=AF.Sin, bias=halfpi[:]),
                         reads=[Bkf, B_negpi], writes=[Btab[kd * 2]])
                P.op("vector", lambda e: e.memset(ss[:], 0.0), reads=Btab, writes=Bss)

            if "stopT" in dbg:
                P.run_block()
                return
            def emit_load(tt):
                xb, xsem = xring.next()
                P.dma("sync", xsem, xt[tt % 2][:], x_src[tt * 128:(tt + 1) * 128, :], writes=[xb])
                return xb

            xb_next = emit_load(0)
            for tt in range(8):
                xb = xb_next
                if tt + 1 < 8:
                    xb_next = emit_load(tt + 1)
                b2 = tt % 2
                P.op("scalar", lambda e, tt=tt, b2=b2: e.activation(
                    out=junk[:], in_=xt[b2][:], func=AF.Square, accum_out=ss[:, tt:tt + 1]),
                    reads=[xb], writes=[Bjunk, Bss[tt]])
                P.op("scalar", lambda e, tt=tt: e.activation(
                    out=rstd[:, tt:tt + 1], in_=ss[:, tt:tt + 1], func=AF.Sqrt, scale=1.0 / D, bias=epsT[:]),
                    reads=[Bss[tt], B_eps], writes=[Brstd[tt]])
                P.op("vector", lambda e, tt=tt: e.reciprocal(out=rstd[:, tt:tt + 1], in_=rstd[:, tt:tt + 1]),
                    reads=[Brstd[tt]], writes=[Brstd[tt]])
                P.op("vector", lambda e, tt=tt, b2=b2: e.tensor_scalar(
                    out=xs[b2][:], in0=xt[b2][:], scalar1=rstd[:, tt:tt + 1], scalar2=None, op0=ALU.mult),
                    reads=[xb, Brstd[tt]], writes=[Bxs[b2]])
                for k in range(KC):
                    P.op("tensor", lambda e, b2=b2, k=k: e.transpose(
                        out=tp[b2][:, k, :], in_=xs[b2][:, k * 128:(k + 1) * 128], identity=ident_b[:]),
                        reads=[Bxs[b2], B_idb], writes=[Btp[b2]] if k == 0 else [], mark=(k == KC - 1))
                for k in range(KC):
                    P.op("scalar", lambda e, b2=b2, k=k, tt=tt: e.activation(
                        out=hT[:, k, tt * 128:(tt + 1) * 128], in_=tp[b2][:, k, :], func=AF.Identity,
                        scale=modA[:, 0, k:k + 1], bias=modA[:, 1, k:k + 1]),
                        reads=[Btp[b2], B_mod], writes=[BhT[tt]] if k == 0 else [], mark=True)
                BhT[tt].w = Ev(P.done["scalar"], P.done["scalar"].n)

            if "stopS1" in dbg:
                P.run_block()
                return
            items = []
            for (c0, ncols, jobs) in wblocks:
                for tc in range(2):
                    for job in jobs:
                        if job[0] == "fm":
                            items.append((c0, ncols, job, tc, None))
                        else:
                            for t4 in range(4):
                                items.append((c0, ncols, job, tc, t4))
            cur_w = {"key": None, "buf": None, "idx": -1}
            loaded = {}
            wcount = [0]

            def ensure_w(c0, ncols):
                if c0 not in loaded:
                    wb, wsem = wring.next()
                    i = wcount[0] % 2
                    wcount[0] += 1
                    P.dma("gpsimd", wsem, wbuf[i][:, :, 0:ncols], w_in_v[:, :, c0:c0 + ncols], writes=[wb])
                    loaded[c0] = (wb, i)
                return loaded[c0]

            order = [b[0] for b in wblocks]
            npipe = [0]

            def main(item):
                c0, ncols, job, tc, t4 = item
                wb, wi = ensure_w(c0, ncols)
                bi = order.index(c0)
                if bi + 1 < len(order):
                    nb = wblocks[bi + 1]
                    ensure_w(nb[0], nb[1])
                n = npipe[0]
                npipe[0] += 1
                b2 = n % 2
                rec = {"b2": b2, "item": item}
                if job[0] == "fm":
                    _, off, M, kind, dst = job
                    hbufs = BhT[tc * 4:(tc + 1) * 4]
                    for k in range(KC):
                        P.op("tensor", lambda e, b2=b2, wi=wi, k=k, off=off, M=M, tc=tc: e.matmul(
                            p1[b2][0:M, :], lhsT=wbuf[wi][:, k, off:off + M], rhs=hT[:, k, tc * 512:(tc + 1) * 512],
                            start=(k == 0), stop=(k == KC - 1)),
                            reads=[wb] + hbufs, writes=[Bp1[b2]] if k == 0 else [], mark=(k == KC - 1))
                else:
                    off = job[1]
                    N = 16 if job[0] == "iw" else job[2]
                    tt = tc * 4 + t4
                    for k in range(KC):
                        P.op("tensor", lambda e, b2=b2, wi=wi, k=k, off=off, N=N, tt=tt: e.matmul(
                            p1[b2][:, 0:N], lhsT=hT[:, k, tt * 128:(tt + 1) * 128], rhs=wbuf[wi][:, k, off:off + N],
                            start=(k == 0), stop=(k == KC - 1)),
                            reads=[wb, BhT[tt]], writes=[Bp1[b2]] if k == 0 else [], mark=(k == KC - 1))
                return rec

            def tail(rec):
                b2 = rec["b2"]
                c0, ncols, job, tc, t4 = rec["item"]
                if job[0] == "fm":
                    _, off, M, kind, dst = job
                    P.op("vector", lambda e, b2=b2, M=M, kind=kind, tc=tc: e.tensor_tensor(
                        out=t1s[b2][0:M, :], in0=p1[b2][0:M, :], in1=tabs[kind * 2][0:M, tc * 512:(tc + 1) * 512],
                        op=ALU.mult), reads=[Bp1[b2], Btab[kind * 2]], writes=[Bt1[b2]])
                    P.op("vector", lambda e, b2=b2, M=M, kind=kind, tc=tc: e.tensor_tensor(
                        out=t2s[b2][0:M, :], in0=p1[b2][0:M, :], in1=tabs[kind * 2 + 1][0:M, tc * 512:(tc + 1) * 512],
                        op=ALU.mult), reads=[Bp1[b2], Btab[kind * 2 + 1]], writes=[Bt2[b2]])
                    P.op("tensor", lambda e, b2=b2, M=M: e.matmul(
                        p2[b2][0:M, :], lhsT=ident_b[0:M, 0:M], rhs=t1s[b2][0:M, :], start=True, stop=False),
                        reads=[Bt1[b2], B_idb], writes=[Bp2[b2]], mark=False)
                    P.op("tensor", lambda e, b2=b2, M=M, kind=kind: e.matmul(
                        p2[b2][0:M, :], lhsT=rm_b[0:M, kind, 0:M], rhs=t2s[b2][0:M, :], start=False, stop=True),
                        reads=[Bt2[b2], B_rmb], writes=[], mark=True)
                    sbuf_, ssem = sring.next()
                    si = (sring.k - 1) % 3
                    P.op("scalar", lambda e, b2=b2, M=M, si=si: e.activation(
                        out=stg[si][0:M, :], in_=p2[b2][0:M, :], func=AF.Copy),
                        reads=[Bp2[b2]], writes=[sbuf_])
                    P.dma("sync", ssem, dst[:, tc * 512:(tc + 1) * 512], stg[si][0:M, :], reads=[sbuf_])
                elif job[0] == "tm":
                    _, off, N, dst = job
                    tt = tc * 4 + t4
                    sbuf_, ssem = sring.next()
                    si = (sring.k - 1) % 3
                    P.op("scalar", lambda e, b2=b2, N=N, si=si: e.activation(
                        out=stg[si][:, 0:N], in_=p1[b2][:, 0:N], func=AF.Copy),
                        reads=[Bp1[b2]], writes=[sbuf_])
                    P.dma("sync", ssem, dst[tt * 128:(tt + 1) * 128, :], stg[si][:, 0:N], reads=[sbuf_])
                else:
                    tt = tc * 4 + t4
                    P.op("scalar", lambda e, b2=b2, tt=tt: e.activation(
                        out=iw_sb[:, tt, :], in_=p1[b2][:, 0:16], func=AF.Copy, scale=float(IDX_W_SCALE)),
                        reads=[Bp1[b2]], writes=[B_iw])

            prev = None
            for item in items:
                rec = main(item)
                if prev is not None:
                    tail(prev)
                prev = rec
            tail(prev)
            tails = [Ev(s, s.n) for s in sring.sems]
            P.run_block(final_waits=tails)
            xring.release()
            wring.release()
            sring.release()
            P.put_dma_sems(psem)

    for s_ in range(4):
        t0 = s_ * 1024
        blocks = []
        for hb in range(2):
            jobs = [("fm", hh * 128, 128, 0, kdT_scr[hb * 4 + hh][:, t0:t0 + 1024]) for hh in range(4)]
            blocks.append((O_DK + hb * 512, 512, jobs))
        for cb in range(2):
            blocks.append((O_DV + cb * 512, 512, [("tm", 0, 512, vd_scr[t0:t0 + 1024, cb * 512:(cb + 1) * 512])]))
        blocks.append((O_SK, 256, [("fm", 0, 128, 1, skT_scr[:, t0:t0 + 1024]),
                                   ("tm", 128, 128, sv_scr[t0:t0 + 1024, :])]))
        blocks.append((O_IK, 64, [("fm", 0, 64, 0, ikT_scr[:, t0:t0 + 1024])]))
        for d_ in dbg:
            if d_.startswith("blk:"):
                blocks = [blocks[int(v)] for v in d_[4:].split(",")]
        project_set("k%d" % s_, x_all[t0:t0 + 1024, :], pos_all[0:1, t0:t0 + 1024], blocks)
        if "stopB1" in dbg:
            top.close()
            return nc
    blocks = []
    for hb in range(2):
        blocks.append((O_DQ + hb * 512, 512, [("fm", hh * 128, 128, 0, qdT_scr[hb * 4 + hh]) for hh in range(4)]))
    for hb in range(2):
        blocks.append((O_SQ + hb * 512, 512, [("fm", hh * 128, 128, 1, sqT_scr[hb * 4 + hh]) for hh in range(4)]))
    for hb in range(2):
        blocks.append((O_IQ + hb * 512, 512, [("fm", hh * 128, 128, 0, iqT_scr[hb * 4 + hh]) for hh in range(4)]))
    blocks.append((O_IW, 16, [("iw", 0)]))
    project_set("q", x_own, pos_own, blocks)
    if "stopB" in dbg:
        top.close()
        return nc

    NTL = [slot_ntiles(i) for i in range(8)]
    TBASE = [sum(NTL[:i]) for i in range(8)]
    attn_stack = ExitStack()
    mask_stack = ExitStack()
    mixT = sb("mixT", [128, 16, TOWN], BF16, attn_stack)
    B_mix = [Buf() for _ in range(16)]
    maskT_sb = sb("maskT_sb", [128, 144, 128], BF16, mask_stack)
    B_maskT = [Buf() for _ in range(8)]

    with ExitStack() as st:
        IQr = sb("IQr", [128, 8 * 4 * 4 * 128], BF16, st)
        iqst = [sb("iqst%d" % i, [64, TOWN], BF16, st) for i in range(2)]
        ikT = sb("ikT", [128, S], BF16, st)
        dmask = sb("dmask", [128, 32], F32, st)
        bisc = sb("bisc", [128, 2 * NBIS], F32, st)
        maskadd = sb("maskadd", [128, 8, 512], F32, st)
        WT = [sb("WT%d" % i, [128, 16, 32], BF16, st) for i in range(2)]
        wselM = [sb("wselM%d" % i, [128, 4, 4, 128], BF16, st) for i in range(2)]
        Rb = [sb("Rb%d" % i, [128, 512], BF16, st) for i in range(3)]
        scores = [sb("scores%d" % i, [128, S], F32, st) for i in range(2)]
        mask_sb = sb("mask_sb", [128, S], BF16, st)
        junk = mask_sb
        sm = sb("smallc", [128, 16], F32, st)
        wtab = sb("wtab", [128, 2 * NBIS], F32, st)
        wsel_ps = ps("wsel_ps", [128, 4, 128], BF16, st)
        L_ps = [ps("L_ps%d" % i, [128, 512], F32, st) for i in range(3)]
        sc_ps = [ps("sc_ps%d" % i, [128, 512], F32, st) for i in range(2)]
        mt_ps = ps("mt_ps", [128, 4, 128], BF16, st)
        sems = P.get_dma_sems(6)
        BIQ, Bik, Bdm, Bbis, Bmadd = [Buf() for _ in range(5)]
        Bik0 = Buf()
        BWT, BwA = [Buf(), Buf()], [Buf(), Buf()]
        Bwps, Bmtps = Buf(), Buf()
        BR, BL = [Buf() for _ in range(3)], [Buf() for _ in range(3)]
        Bsc, Bscore = [Buf(), Buf()], [Buf(), Buf()]
        Bjunk, Bmask, Bsm, Bwtab = Buf(), Buf(), Buf(), Buf()
        iqring = Ring(P, [Buf(), Buf()])
        IQr_v = IQr[0:64, :].rearrange("d (i j a h q) -> d i j a h q", i=8, j=4, a=4, h=4, q=32)
        P.op("vector", lambda e: e.memset(IQr[64:128, :], 0.0), writes=[BIQ])
        P.op("vector", lambda e: e.memset(ikT[64:128, :], 0.0), writes=[Bik0])
        for w_ in range(2):
            P.op("vector", lambda e, w_=w_: e.memset(wselM[w_][:], 0.0), writes=[BwA[w_]])
        for pr in range(8):
            for hp in range(2):
                hd = 2 * pr + hp
                ib, isem = iqring.next()
                P.dma("sync", isem, iqst[hd % 2][:], iqT_scr[pr, hp * 64:(hp + 1) * 64, :], writes=[ib])
                P.op("vector", lambda e, hd=hd: e.tensor_copy(
                    out=IQr_v[:, :, :, hd // 4, hd % 4, :],
                    in_=iqst[hd % 2][:].rearrange("d (i j q) -> d i j q", i=8, j=4, q=32)),
                    reads=[ib], writes=[BIQ])
        iq_evs = []
        P.dma("sync", sems[2], ikT[0:64, :], ikT_scr, writes=[Bik])
        P.dma("sync", sems[3], dmask[:], cst_dmask, writes=[Bdm])
        P.dma("sync", sems[4], bisc[:], cst_bis, writes=[Bbis])
        P.dma("sync", sems[5], maskadd[:], cst_maskadd, writes=[Bmadd])

        for i in range(8):
            n = NTL[i]
            nch = n // 4
            sbi = i % 2
            P.op("vector", lambda e, i=i, sbi=sbi: e.tensor_tensor(
                out=WT[sbi][:], in0=dmask[:].unsqueeze(1).to_broadcast([128, 16, 32]),
                in1=iw_sb[:, i, :].unsqueeze(2).to_broadcast([128, 16, 32]), op=ALU.mult),
                reads=[Bdm, B_iw], writes=[BWT[sbi]])
            for a in range(4):
                P.op("tensor", lambda e, a=a, sbi=sbi: e.transpose(
                    out=wsel_ps[:, a, :], in_=WT[sbi][:, 4 * a:4 * a + 4, :], identity=ident_b[:]),
                    reads=[BWT[sbi], B_idb], writes=[Bwps] if a == 0 else [], mark=(a == 3))
            for j in range(4):
                P.op("scalar", lambda e, sbi=sbi, j=j: e.activation(
                    out=wselM[sbi][:, j, :, 32 * j:32 * j + 32], in_=wsel_ps[:, :, 32 * j:32 * j + 32], func=AF.Copy),
                    reads=[Bwps], writes=[BwA[sbi]])
            cnt = [0]

            def Lmm(c, j, a, i=i):
                lb = cnt[0] % 3
                cnt[0] += 1
                P.op("tensor", lambda e, lb=lb, c=c, j=j, a=a, i=i: e.matmul(
                    L_ps[lb][:], lhsT=IQr[:, ((i * 4 + j) * 4 + a) * 128:((i * 4 + j) * 4 + a) * 128 + 128],
                    rhs=ikT[:, c * 512:(c + 1) * 512], start=True, stop=True),
                    reads=[BIQ, Bik, Bik0], writes=[BL[lb]], waits=iq_evs, mark=True)
                P.op("scalar", lambda e, lb=lb: e.activation(out=Rb[lb][:], in_=L_ps[lb][:], func=AF.Relu),
                     reads=[BL[lb]], writes=[BR[lb]])
                return lb

            def Smm(c, j, a, lb, sp, sbi=sbi):
                P.op("tensor", lambda e, lb=lb, j=j, a=a, sp=sp, sbi=sbi: e.matmul(
                    sc_ps[sp][:], lhsT=wselM[sbi][:, j, a, :], rhs=Rb[lb][:],
                    start=(j == 0 and a == 0), stop=(j == 3 and a == 3)),
                    reads=[BR[lb], BwA[sbi]], writes=[Bsc[sp]] if (j == 0 and a == 0) else [],
                    mark=True)

            for c in range(nch):
                sp = c % 2
                work = [(j, a) for j in range(4) for a in range(4)]
                prev = None
                for (j, a) in work:
                    lb = Lmm(c, j, a)
                    if prev is not None:
                        Smm(c, prev[0], prev[1], prev[2], sp)
                    prev = (j, a, lb)
                Smm(c, prev[0], prev[1], prev[2], sp)
                Bsc[sp].w = Ev(P.done["tensor"], P.done["tensor"].n)
                P.op("scalar", lambda e, sp=sp, c=c, sbi=sbi: e.activation(
                    out=scores[sbi][:, c * 512:(c + 1) * 512], in_=sc_ps[sp][:], func=AF.Copy),
                    reads=[Bsc[sp]], writes=[Bscore[sbi]])
            W = n * 128
            P.op("vector", lambda e, sbi=sbi, W=W: e.tensor_reduce(out=sm[:, 1:2], in_=scores[sbi][:, 0:W], axis=AX.X, op=ALU.max),
                 reads=[Bscore[sbi]], writes=[Bsm])
            P.op("vector", lambda e, sbi=sbi, W=W: e.tensor_reduce(out=sm[:, 0:1], in_=scores[sbi][:, 0:W], axis=AX.X, op=ALU.min),
                 reads=[Bscore[sbi]], writes=[Bsm])
            P.op("vector", lambda e, sbi=sbi, W=W, i=i: e.tensor_tensor(
                out=scores[sbi][:, W - 512:W], in0=scores[sbi][:, W - 512:W], in1=maskadd[:, i, :], op=ALU.add),
                reads=[Bmadd, Bscore[sbi]], writes=[Bscore[sbi]])
            P.op("vector", lambda e: e.tensor_tensor(out=sm[:, 2:3], in0=sm[:, 1:2], in1=sm[:, 0:1], op=ALU.subtract),
                 reads=[Bsm], writes=[Bsm])
            P.op("vector", lambda e: e.tensor_scalar(out=wtab[:], in0=bisc[:], scalar1=sm[:, 2:3], scalar2=None, op0=ALU.mult),
                 reads=[Bsm, Bbis], writes=[Bwtab])
            P.op("vector", lambda e: e.tensor_tensor(out=sm[:, 3:4], in0=sm[:, 0:1], in1=wtab[:, 0:1], op=ALU.add),
                 reads=[Bsm, Bwtab], writes=[Bsm])
            for it in range(NBIS):
                P.op("vector", lambda e, sbi=sbi, W=W: e.tensor_scalar(
                    out=junk[:, 0:W], in0=scores[sbi][:, 0:W], scalar1=sm[:, 3:4], scalar2=0.0,
                    op0=ALU.is_ge, op1=ALU.add, accum_out=sm[:, 4:5]),
                    reads=[Bscore[sbi], Bsm], writes=[Bmask, Bsm])
                P.op("vector", lambda e, it=it: e.scalar_tensor_tensor(
                    out=sm[:, 5:6], in0=sm[:, 4:5], scalar=255.5, in1=wtab[:, it:it + 1], op0=ALU.is_ge, op1=ALU.mult),
                    reads=[Bsm, Bwtab], writes=[Bsm])
                P.op("vector", lambda e, it=it: e.scalar_tensor_tensor(
                    out=sm[:, 3:4], in0=sm[:, 5:6], scalar=wtab[:, NBIS + it:NBIS + it + 1], in1=sm[:, 3:4],
                    op0=ALU.subtract, op1=ALU.add),
                    reads=[Bsm, Bwtab], writes=[Bsm])
            P.op("vector", lambda e, sbi=sbi, W=W: e.tensor_scalar(
                out=mask_sb[:, 0:W], in0=scores[sbi][:, 0:W], scalar1=sm[:, 3:4], scalar2=None, op0=ALU.is_ge),
                reads=[Bscore[sbi], Bsm], writes=[Bmask])
            for g4 in range(nch):
                for t4 in range(4):
                    kt = g4 * 4 + t4
                    P.op("tensor", lambda e, kt=kt, t4=t4: e.transpose(
                        out=mt_ps[:, t4, :], in_=mask_sb[:, kt * 128:(kt + 1) * 128], identity=ident_b[:]),
                        reads=[Bmask, B_idb], writes=[Bmtps] if t4 == 0 else [], mark=(t4 == 3))
                P.op("scalar", lambda e, g4=g4, i=i: e.activation(
                    out=maskT_sb[:, TBASE[i] + g4 * 4:TBASE[i] + g4 * 4 + 4, :], in_=mt_ps[:], func=AF.Copy),
                    reads=[Bmtps], writes=[B_maskT[i]])
        if "dbgC" in dbg:
            dsem = P.get_dma_sems(1)
            P.dma("sync", dsem[0], dbg_scr[:, 0:4096], scores[1][:], reads=[Bscore[1]])
            P.run_block(final_waits=[Ev(dsem[0], dsem[0].n)])
        else:
            P.run_block()
        P.put_dma_sems(sems)
    if "stopC" in dbg:
        mask_stack.close(); attn_stack.close(); top.close()
        return nc

    with ExitStack() as st:
        sqT = sb("sqT", [128, 8, TOWN], BF16, st)
        skT = sb("skT", [128, S], BF16, st)
        svv = sb("svv", [128, NT, 128], BF16, st)
        Eb = [sb("Eb%d" % i, [128, 512], BF16, st) for i in range(3)]
        Pm = [sb("Pm%d" % i, [128, 512], BF16, st) for i in range(3)]
        lnb = sb("lnb", [128, 512], F32, st)
        rinv = sb("rinvd", [128, 512], F32, st)
        st_ps = [ps("st_ps%d" % i, [128, 512], F32, st) for i in range(3)]
        O_ps = [ps("O_ps%d" % i, [128, 512], F32, st) for i in range(2)]
        s_ps = [ps("s_ps%d" % i, [128, 512], F32, st) for i in range(2)]
        sems = P.get_dma_sems(10)
        Bsq = [Buf() for _ in range(8)]
        Bsk, Bsv = Buf(), Buf()
        BE, BPm, Bst = ([Buf() for _ in range(3)] for _ in range(3))
        BO, BS = [Buf(), Buf()], [Buf(), Buf()]
        Bln, Brinv = Buf(), Buf()
        for h in range(8):
            P.dma("sync", sems[h], sqT[:, h, :], sqT_scr[h], writes=[Bsq[h]])
        P.dma("sync", sems[8], skT[:], skT_scr, writes=[Bsk])
        P.dma("gpsimd", sems[9], svv[:], sv_scr.rearrange("(t p) d -> p t d", p=128), writes=[Bsv])
        SC = 128 ** -0.5
        cnt = [0]
        grp = 0
        for i in range(8):
            n = NTL[i]
            for hh in range(2):
                ob = grp % 2
                grp += 1

                def qk(kt, i=i, hh=hh):
                    b3 = cnt[0] % 3
                    cnt[0] += 1
                    P.op("tensor", lambda e, b3=b3, kt=kt, i=i, hh=hh: e.matmul(
                        st_ps[b3][:], lhsT=skT[:, kt * 128:(kt + 1) * 128],
                        rhs=sqT[:, 4 * hh:4 * hh + 4, i * 128:(i + 1) * 128], start=True, stop=True),
                        reads=[Bsk] + Bsq[4 * hh:4 * hh + 4], writes=[Bst[b3]], mark=True)
                    P.op("scalar", lambda e, b3=b3: e.activation(out=Eb[b3][:], in_=st_ps[b3][:], func=AF.Exp, scale=SC),
                         reads=[Bst[b3]], writes=[BE[b3]])
                    P.op("vector", lambda e, b3=b3, kt=kt, i=i: e.tensor_tensor(
                        out=Pm[b3][:].rearrange("p (h q) -> p h q", h=4),
                        in0=Eb[b3][:].rearrange("p (h q) -> p h q", h=4),
                        in1=maskT_sb[:, TBASE[i] + kt, :].unsqueeze(1).to_broadcast([128, 4, 128]), op=ALU.mult),
                        reads=[BE[b3], B_maskT[i]], writes=[BPm[b3]])
                    return b3

                def pv(kt, b3, ob=ob, n=n):
                    P.op("tensor", lambda e, b3=b3, kt=kt, ob=ob, n=n: e.matmul(
                        O_ps[ob][:], lhsT=svv[:, kt, :], rhs=Pm[b3][:], start=(kt == 0), stop=(kt == n - 1)),
                        reads=[Bsv, BPm[b3]], writes=[BO[ob]] if kt == 0 else [], mark=False)
                    P.op("tensor", lambda e, b3=b3, kt=kt, ob=ob, n=n: e.matmul(
                        s_ps[ob][:], lhsT=ones_b[:], rhs=Pm[b3][:], start=(kt == 0), stop=(kt == n - 1)),
                        reads=[B_ones, BPm[b3]], writes=[BS[ob]] if kt == 0 else [], mark=True)

                prev = None
                for kt in range(n):
                    b3 = qk(kt)
                    if prev is not None:
                        pv(prev[0], prev[1])
                    prev = (kt, b3)
                pv(prev[0], prev[1])
                lastev = Ev(P.done["tensor"], P.done["tensor"].n)
                BO[ob].w = lastev
                BS[ob].w = lastev
                P.op("scalar", lambda e, ob=ob: e.activation(out=lnb[:], in_=s_ps[ob][:], func=AF.Ln),
                     reads=[BS[ob]], writes=[Bln])
                P.op("scalar", lambda e: e.activation(out=rinv[:], in_=lnb[:], func=AF.Exp, scale=-1.0),
                     reads=[Bln], writes=[Brinv])
                P.op("vector", lambda e, ob=ob, i=i, hh=hh: e.tensor_tensor(
                    out=mixT[:, 8 + 4 * hh:8 + 4 * hh + 4, i * 128:(i + 1) * 128],
                    in0=O_ps[ob][:].rearrange("p (h q) -> p h q", h=4),
                    in1=rinv[:].rearrange("p (h q) -> p h q", h=4), op=ALU.mult),
                    reads=[BO[ob], Brinv], writes=B_mix[8 + 4 * hh:8 + 4 * hh + 4])
        P.run_block()
        P.put_dma_sems(sems)
    mask_stack.close()
    if "stopD" in dbg:
        attn_stack.close(); top.close()
        return nc

    with ExitStack() as st:
        qd = [[sb("qd%d_%d" % (i, m), [128, TOWN], BF16, st) for m in range(2)] for i in range(2)]
        kd = [sb("kd%d" % i, [128, S], BF16, st) for i in range(2)]
        vd = [sb("vd%d" % i, [128, NT, 128], BF16, st) for i in range(2)]
        cmask = sb("cmask", [128, 32, 128], BF16, st)
        Eb = [sb("Ee%d" % i, [128, 2, 512], BF16, st) for i in range(3)]
        lamv = sb("lamv", [128, 4, 64], F32, st)
        lsm = sb("lsm", [128, 8], F32, st)
        lprod = sb("lprod", [128, 64], F32, st)
        gsub = sb("gsub", [128, 1], F32, st)
        f1 = sb("f1", [128, 512], F32, st)
        f2 = sb("f2", [128, 512], F32, st)
        r1 = sb("r1e", [128, 512], F32, st)
        r2 = sb("r2e", [128, 512], F32, st)
        dd = sb("dde", [128, 512], F32, st)
        o1s = sb("o1s", [128, 512], F32, st)
        o2s = sb("o2s", [128, 512], F32, st)
        sqb = sb("sqbe", [128, 512], BF16, st)
        rs = sb("rse", [128, 512], F32, st)
        sp_ps = [ps("sp_ps%d" % i, [128, 2, 512], F32, st) for i in range(2)]
        O1 = ps("O1", [128, 512], F32, st)
        O2 = ps("O2", [128, 512], F32, st)
        S1 = ps("S1", [128, 512], F32, st)
        S2 = ps("S2", [128, 512], F32, st)
        sems = P.get_dma_sems(12)
        Bcm, Blam, Blsm, Bgs = Buf(), [Buf() for _ in range(4)], Buf(), Buf()
        P.dma("gpsimd", sems[0], cmask[:], cst_maskT, writes=[Bcm])
        for n_, lv in enumerate([lam_q1, lam_k1, lam_q2, lam_k2]):
            P.dma("sync", sems[1 + n_], lamv[:, n_, :], lv.to_broadcast([128, 64]), writes=[Blam[n_]])
        P.dma("sync", sems[5], gsub[:], g_diff_sub.rearrange("o p -> p o"), writes=[Bgs])
        Blp = Buf()
        for m_ in range(2):
            P.op("vector", lambda e, m_=m_: e.tensor_tensor(out=lprod[:], in0=lamv[:, 2 * m_, :], in1=lamv[:, 2 * m_ + 1, :], op=ALU.mult),
                 reads=[Blam[2 * m_], Blam[2 * m_ + 1]], writes=[Blp])
            P.op("vector", lambda e, m_=m_: e.tensor_reduce(out=lsm[:, m_:m_ + 1], in_=lprod[:], axis=AX.X, op=ALU.add),
                 reads=[Blp], writes=[Blsm])
        P.op("scalar", lambda e: e.activation(out=lsm[:, 2:4], in_=lsm[:, 0:2], func=AF.Exp), reads=[Blsm], writes=[Blsm])
        P.op("vector", lambda e: e.scalar_tensor_tensor(out=lsm[:, 4:5], in0=lsm[:, 3:4], scalar=float(LAM_INIT), in1=lsm[:, 2:3],
                                                        op0=ALU.subtract, op1=ALU.subtract), reads=[Blsm], writes=[Blsm])
        P.op("vector", lambda e: e.tensor_scalar(out=lsm[:, 5:6], in0=gsub[:], scalar1=float(1.0 - LAM_INIT), scalar2=None, op0=ALU.mult),
             reads=[Bgs, Blsm], writes=[Blsm])
        Bqd, Bkd, Bvd = [Buf(), Buf()], [Buf(), Buf()], [Buf(), Buf()]
        BE, Bsp = [Buf() for _ in range(3)], [Buf(), Buf()]
        BO1, BO2, BS1, BS2 = Buf(), Buf(), Buf(), Buf()
        Bf1, Bf2, Br1, Br2, Bdd, Bsqb, Brs = [Buf() for _ in range(7)]
        Bo1s, Bo2s = Buf(), Buf()
        deferred = [None]
        SCD = 64 ** -0.5

        Bqz = Buf()
        for i_ in range(2):
            for m_ in range(2):
                P.op("vector", lambda e, i_=i_, m_=m_: e.memset(qd[i_][m_][:], 0.0), writes=[Bqz])

        def load_head(h):
            hb = h % 2
            P.dma("sync", sems[6 + hb], qd[hb][0][0:64, :], qdT_scr[h][0:64, :], reads=[Bqz], writes=[Bqd[hb]])
            P.dma("sync", sems[6 + hb], qd[hb][1][64:128, :], qdT_scr[h][64:128, :], reads=[Bqz], writes=[Bqd[hb]])
            P.dma("sync", sems[8 + hb], kd[hb][:], kdT_scr[h], writes=[Bkd[hb]])
            P.dma("gpsimd", sems[10 + hb], vd[hb][:], vd_scr[:, h * 128:(h + 1) * 128].rearrange("(t p) d -> p t d", p=128),
                  writes=[Bvd[hb]])

        load_head(0)
        ecnt = [0]
        for h in range(8):
            hb = h % 2
            if h + 1 < 8:
                load_head(h + 1)
            for qc in range(2):
                slots = list(range(4 * qc, 4 * qc + 4))
                nmax = NTL[slots[-1]]

                def c0_of(kt, slots=slots):
                    first = sum(1 for s_ in slots if NTL[s_] <= kt)
                    return 128 * first

                def qk(kt, hb=hb, qc=qc, slots=slots):
                    c0 = c0_of(kt)
                    sb2 = ecnt[0] % 2
                    eb = ecnt[0] % 3
                    ecnt[0] += 1
                    for m_ in range(2):
                        P.op("tensor", lambda e, m_=m_, sb2=sb2, kt=kt, c0=c0, hb=hb, qc=qc: e.matmul(
                            sp_ps[sb2][:, m_, c0:512], lhsT=kd[hb][:, kt * 128:(kt + 1) * 128],
                            rhs=qd[hb][m_][:, qc * 512 + c0:(qc + 1) * 512], start=True, stop=True),
                            reads=[Bkd[hb], Bqd[hb]], writes=[Bsp[sb2]] if m_ == 0 else [], mark=(m_ == 1))
                    P.op("scalar", lambda e, sb2=sb2, eb=eb, c0=c0: e.activation(
                        out=Eb[eb][:, :, c0:512], in_=sp_ps[sb2][:, :, c0:512], func=AF.Exp, scale=SCD),
                        reads=[Bsp[sb2]], writes=[BE[eb]])
                    for lc, s_ in enumerate(slots):
                        nn = NTL[s_]
                        if nn - 4 <= kt < nn:
                            jj = kt - (nn - 4)
                            P.op("vector", lambda e, eb=eb, lc=lc, s_=s_, jj=jj: e.tensor_tensor(
                                out=Eb[eb][:, :, lc * 128:(lc + 1) * 128], in0=Eb[eb][:, :, lc * 128:(lc + 1) * 128],
                                in1=cmask[:, s_ * 4 + jj, :].unsqueeze(1).to_broadcast([128, 2, 128]), op=ALU.mult),
                                reads=[BE[eb], Bcm], writes=[BE[eb]])
                    return (eb, c0)

                def pv(kt, eb, c0, hb=hb, nmax=nmax):
                    first = (kt == 0)
                    last = (kt == nmax - 1)
                    for m_, (Ot, St, BOt, BSt) in enumerate([(O1, S1, BO1, BS1), (O2, S2, BO2, BS2)]):
                        P.op("tensor", lambda e, m_=m_, Ot=Ot, eb=eb, c0=c0, kt=kt, hb=hb, first=first, last=last: e.matmul(
                            Ot[:, c0:512], lhsT=vd[hb][:, kt, :], rhs=Eb[eb][:, m_, c0:512], start=first, stop=last,
                            skip_group_check=True),
                            reads=[Bvd[hb], BE[eb]], writes=[BOt] if first else [], mark=False)
                        P.op("tensor", lambda e, m_=m_, St=St, eb=eb, c0=c0, first=first, last=last: e.matmul(
                            St[:, c0:512], lhsT=ones_b[:], rhs=Eb[eb][:, m_, c0:512], start=first, stop=last,
                            skip_group_check=True),
                            reads=[B_ones, BE[eb]], writes=[BSt] if first else [], mark=(m_ == 1))

                prev = None
                for kt in range(nmax):
                    cur = qk(kt)
                    if prev is not None:
                        pv(prev[0], prev[1], prev[2])
                    prev = (kt, cur[0], cur[1])
                    if kt == 4 and deferred[0] is not None:
                        deferred[0]()
                        deferred[0] = None
                pv(prev[0], prev[1], prev[2])
                lastev = Ev(P.done["tensor"], P.done["tensor"].n)
                for B_ in (BO1, BO2, BS1, BS2):
                    B_.w = lastev
                P.op("vector", lambda e: e.tensor_copy(out=o1s[:], in_=O1[:]), reads=[BO1], writes=[Bo1s])
                P.op("vector", lambda e: e.tensor_copy(out=o2s[:], in_=O2[:]), reads=[BO2], writes=[Bo2s])
                P.op("scalar", lambda e: e.activation(out=f1[:], in_=S1[:], func=AF.Ln), reads=[BS1], writes=[Bf1])
                P.op("scalar", lambda e: e.activation(out=f2[:], in_=S2[:], func=AF.Ln), reads=[BS2], writes=[Bf2])
                P.op("scalar", lambda e: e.activation(out=r1[:], in_=f1[:], func=AF.Exp, scale=-1.0), reads=[Bf1], writes=[Br1])
                P.op("scalar", lambda e: e.activation(out=r2[:], in_=f2[:], func=AF.Exp, scale=-1.0), reads=[Bf2], writes=[Br2])
                P.op("vector", lambda e: e.tensor_tensor(out=f1[:], in0=o1s[:], in1=r1[:], op=ALU.mult),
                     reads=[Bo1s, Br1], writes=[Bf1])
                P.op("vector", lambda e: e.tensor_tensor(out=f2[:], in0=o2s[:], in1=r2[:], op=ALU.mult),
                     reads=[Bo2s, Br2], writes=[Bf2])
                P.op("vector", lambda e: e.scalar_tensor_tensor(out=dd[:], in0=f2[:], scalar=lsm[:, 4:5], in1=f1[:],
                                                                op0=ALU.mult, op1=ALU.add),
                     reads=[Bf1, Bf2, Blsm], writes=[Bdd])
                P.op("scalar", lambda e: e.activation(out=sqb[:], in_=dd[:], func=AF.Square), reads=[Bdd], writes=[Bsqb])

                def part2(h=h, qc=qc):
                    msb = ecnt[0] % 2
                    ecnt[0] += 1
                    P.op("tensor", lambda e, msb=msb: e.matmul(sp_ps[msb][:, 0, :], lhsT=ones_b[:], rhs=sqb[:], start=True, stop=True),
                         reads=[Bsqb, B_ones], writes=[Bsp[msb]], mark=True)
                    P.op("scalar", lambda e, msb=msb: e.activation(out=rs[:], in_=sp_ps[msb][:, 0, :], func=AF.Sqrt,
                                                                   scale=1.0 / 128.0, bias=epsT[:]),
                         reads=[Bsp[msb], B_eps], writes=[Brs])
                    P.op("vector", lambda e: e.reciprocal(out=rs[:], in_=rs[:]), reads=[Brs], writes=[Brs])
                    P.op("vector", lambda e, h=h, qc=qc: e.scalar_tensor_tensor(
                        out=mixT[:, h, qc * 512:(qc + 1) * 512], in0=dd[:], scalar=lsm[:, 5:6], in1=rs[:],
                        op0=ALU.mult, op1=ALU.mult), reads=[Bdd, Brs, Blsm], writes=[B_mix[h]])

                deferred[0] = part2
        if deferred[0] is not None:
            deferred[0]()
            deferred[0] = None
        if "dbgE" in dbg:
            dsem = P.get_dma_sems(1)
            mixf = sb("mixf", [128, 4096], F32, st)
            Bmf = Buf()
            P.op("vector", lambda e: e.tensor_copy(out=mixf[:, 0:1024], in_=mixT[:, 0, :]), reads=B_mix, writes=[Bmf])
            P.op("vector", lambda e: e.tensor_copy(out=mixf[:, 1024:2048], in_=mixT[:, 5, :]), reads=B_mix, writes=[Bmf])
            P.op("vector", lambda e: e.tensor_copy(out=mixf[:, 2048:3072], in_=mixT[:, 8, :]), reads=B_mix, writes=[Bmf])
            P.op("vector", lambda e: e.tensor_copy(out=mixf[:, 3072:4096], in_=mixT[:, 15, :]), reads=B_mix, writes=[Bmf])
            P.dma("sync", dsem[0], dbg_scr[:, 0:4096], mixf[:], reads=[Bmf])
            P.run_block(final_waits=[Ev(dsem[0], dsem[0].n)])
        else:
            P.run_block()
        P.put_dma_sems(sems)
    if "stopE" in dbg:
        attn_stack.close(); top.close()
        return nc

    def row_gain(tag, chunk, gvec, stack, sems2):
        ga = sb("ga" + tag, [128, D], F32, stack)
        gt = sb("gt" + tag, [128, D], F32, stack)
        Bga, Bgt = Buf(), Buf()
        P.dma("sync", sems2[0], ga[:], mod_scr[0:1, chunk * D:(chunk + 1) * D].to_broadcast([128, D]), writes=[Bga])
        P.dma("sync", sems2[1], gt[:], gvec.to_broadcast([128, D]), writes=[Bgt])
        P.op("vector", lambda e: e.tensor_tensor(out=ga[:], in0=ga[:], in1=gt[:], op=ALU.mult), reads=[Bga, Bgt], writes=[Bga])
        return ga, Bga

    def norm_residual(tag, stack, src_tiles, Bsrc, ssq, Bssq, nparts, resid_src, ga, Bga, dst):
        xr = [sb("xr%d%s" % (i, tag), [128, D], F32, stack) for i in range(2)]
        og = [sb("og%d%s" % (i, tag), [128, D], F32, stack) for i in range(2)]
        s8 = sb("s8" + tag, [128, 8], F32, stack)
        Bs8 = [Buf() for _ in range(8)]
        xring = Ring(P, [Buf(), Buf()])
        oring = Ring(P, [Buf(), Buf()])
        for tt in range(8):
            xb, xsem = xring.next()
            ob, osem = oring.next()
            b2 = tt % 2
            P.dma("sync", xsem, xr[b2][:], resid_src[tt * 128:(tt + 1) * 128, :], writes=[xb])
            P.op("vector", lambda e, tt=tt: e.tensor_reduce(out=s8[:, tt:tt + 1], in_=ssq[:, tt * 4:tt * 4 + nparts], axis=AX.X, op=ALU.add),
                 reads=[Bssq[tt]], writes=[Bs8[tt]])
            P.op("scalar", lambda e, tt=tt: e.activation(out=s8[:, tt:tt + 1], in_=s8[:, tt:tt + 1], func=AF.Sqrt,
                                                         scale=1.0 / D, bias=epsT[:]),
                 reads=[Bs8[tt], B_eps], writes=[Bs8[tt]])
            P.op("vector", lambda e, tt=tt: e.reciprocal(out=s8[:, tt:tt + 1], in_=s8[:, tt:tt + 1]),
                 reads=[Bs8[tt]], writes=[Bs8[tt]])
            P.op("vector", lambda e, tt=tt, b2=b2: e.scalar_tensor_tensor(
                out=og[b2][:], in0=src_tiles(tt), scalar=s8[:, tt:tt + 1], in1=ga[:], op0=ALU.mult, op1=ALU.mult),
                reads=[Bsrc[tt], Bs8[tt], Bga], writes=[ob])
            P.op("vector", lambda e, b2=b2: e.tensor_tensor(out=og[b2][:], in0=og[b2][:], in1=xr[b2][:], op=ALU.add),
                 reads=[xb, ob], writes=[ob])
            P.dma("sync", osem, dst[tt * 128:(tt + 1) * 128, :], og[b2][:], reads=[ob])
        return [Ev(s_, s_.n) for s_ in oring.sems], [xring, oring]

    with ExitStack() as st:
        wob = [sb("wob%d" % i, [128, KC, 512], BF16, st) for i in range(2)]
        mixed = sb("mixed", [128, 8, D], F32, st)
        ssq = sb("ssqF", [128, 32], F32, st)
        junkf = sb("junkf", [128, 512], BF16, st)
        acc = [ps("accF%d" % i, [128, 512], F32, st) for i in range(4)]
        Bacc = [Buf() for _ in range(4)]
        Bmixed, Bssq = [Buf() for _ in range(8)], [Buf() for _ in range(8)]
        Bjk = Buf()
        wring = Ring(P, [Buf(), Buf()])
        sems2 = P.get_dma_sems(2)
        if "noRG" not in dbg:
            ga, Bga = row_gain("F", 2, g_attn_post, st, sems2)
        w_o_v = w_o.rearrange("(k p) n -> p k n", p=128)
        P.op("vector", lambda e: e.memset(ssq[:], 0.0), writes=Bssq)
        wbs = []
        for cb in range(2):
            wb, wsem = wring.next()
            P.dma("gpsimd", wsem, wob[cb % 2][:], w_o_v[:, :, cb * 512:(cb + 1) * 512], writes=[wb])
            wbs.append(wb)
        na = 0
        for cb in range(4):
            wb = wbs[cb]
            for tt in range(8):
                ab = na % 4
                na += 1
                for k in range(KC):
                    P.op("tensor", lambda e, ab=ab, k=k, tt=tt, cb=cb: e.matmul(
                        acc[ab][:], lhsT=mixT[:, k, tt * 128:(tt + 1) * 128], rhs=wob[cb % 2][:, k, :],
                        start=(k == 0), stop=(k == KC - 1)),
                        reads=[B_mix[k], wb], writes=[Bacc[ab]] if k == 0 else [], mark=(k == KC - 1))
                P.op("vector", lambda e, ab=ab, tt=tt, cb=cb: e.tensor_copy(out=mixed[:, tt, cb * 512:(cb + 1) * 512], in_=acc[ab][:]),
                     reads=[Bacc[ab]], writes=[Bmixed[tt]])
                P.op("scalar", lambda e, tt=tt, cb=cb: e.activation(
                    out=junkf[:], in_=mixed[:, tt, cb * 512:(cb + 1) * 512], func=AF.Square,
                    accum_out=ssq[:, tt * 4 + cb:tt * 4 + cb + 1]),
                    reads=[Bmixed[tt]], writes=[Bjk, Bssq[tt]])
            if cb + 2 < 4:
                wb2, wsem = wring.next()
                P.dma("gpsimd", wsem, wob[cb % 2][:], w_o_v[:, :, (cb + 2) * 512:(cb + 3) * 512], writes=[wb2])
                wbs.append(wb2)
        if "stopF1" in dbg:
            P.run_block()
            st.close(); attn_stack.close(); top.close()
            return nc
        tails, rings = norm_residual("F", st, lambda tt: mixed[:, tt, :], Bmixed, ssq, Bssq, 4, x_own, ga, Bga, x1_scr)
        P.run_block(final_waits=tails)
        for r_ in rings + [wring]:
            r_.release()
        P.put_dma_sems(sems2)
    attn_stack.close()
    if "stopF" in dbg:
        top.close()
        return nc

    ffn_stack = ExitStack()
    aT = sb("aT", [128, NFF, TOWN], BF16, ffn_stack)
    B_aT = [Buf() for _ in range(NFF)]
    with ExitStack() as st:
        h2T = sb("h2T", [128, KC, TOWN], BF16, st)
        Bh2 = [Buf() for _ in range(8)]
        with ExitStack() as st1:
            xt = [sb("xtg%d" % i, [128, D], F32, st1) for i in range(2)]
            xs = [sb("xsg%d" % i, [128, D], BF16, st1) for i in range(2)]
            junk = sb("junkg", [128, D], BF16, st1)
            ss = sb("ssg", [128, 8], F32, st1)
            tp = [ps("tpg%d" % i, [128, KC, 128], BF16, st1) for i in range(2)]
            xring = Ring(P, [Buf(), Buf()])
            Bxs, Btp, Bss, Bjk = [Buf(), Buf()], [Buf(), Buf()], [Buf() for _ in range(8)], Buf()
            P.op("vector", lambda e: e.memset(ss[:], 0.0), writes=Bss)
            for tt in range(8):
                xb, xsem = xring.next()
                b2 = tt % 2
                P.dma("sync", xsem, xt[b2][:], x1_scr[tt * 128:(tt + 1) * 128, :], writes=[xb])
                P.op("scalar", lambda e, tt=tt, b2=b2: e.activation(
                    out=junk[:], in_=xt[b2][:], func=AF.Square, accum_out=ss[:, tt:tt + 1]),
                    reads=[xb], writes=[Bjk, Bss[tt]])
                P.op("scalar", lambda e, tt=tt: e.activation(
                    out=ss[:, tt:tt + 1], in_=ss[:, tt:tt + 1], func=AF.Sqrt, scale=1.0 / D, bias=epsT[:]),
                    reads=[Bss[tt], B_eps], writes=[Bss[tt]])
                P.op("vector", lambda e, tt=tt: e.reciprocal(out=ss[:, tt:tt + 1], in_=ss[:, tt:tt + 1]),
                     reads=[Bss[tt]], writes=[Bss[tt]])
                P.op("vector", lambda e, tt=tt, b2=b2: e.tensor_scalar(
                    out=xs[b2][:], in0=xt[b2][:], scalar1=ss[:, tt:tt + 1], scalar2=None, op0=ALU.mult),
                    reads=[xb, Bss[tt]], writes=[Bxs[b2]])
                for k in range(KC):
                    P.op("tensor", lambda e, b2=b2, k=k: e.transpose(
                        out=tp[b2][:, k, :], in_=xs[b2][:, k * 128:(k + 1) * 128], identity=ident_b[:]),
                        reads=[Bxs[b2], B_idb], writes=[Btp[b2]] if k == 0 else [], mark=(k == KC - 1))
                for k in range(KC):
                    P.op("scalar", lambda e, b2=b2, k=k, tt=tt: e.activation(
                        out=h2T[:, k, tt * 128:(tt + 1) * 128], in_=tp[b2][:, k, :], func=AF.Identity,
                        scale=modA[:, 2, k:k + 1], bias=modA[:, 3, k:k + 1]),
                        reads=[Btp[b2], B_mod], writes=[Bh2[tt]] if k == 0 else [])
                Bh2[tt].w = Ev(P.done["scalar"], P.done["scalar"].n)
            P.run_block()
            xring.release()
        with ExitStack() as st2:
            wg = [sb("wg%d" % i, [128, KC, 256], BF16, st2) for i in range(2)]
            wu = [sb("wu%d" % i, [128, KC, 256], BF16, st2) for i in range(2)]
            sg = [sb("sg%d" % i, [128, 512], F32, st2) for i in range(2)]
            pg = [ps("pg%d" % i, [128, 512], F32, st2) for i in range(2)]
            pu = [ps("pu%d" % i, [128, 512], F32, st2) for i in range(2)]
            Bpg, Bpu, Bsg = [Buf(), Buf()], [Buf(), Buf()], [Buf(), Buf()]
            gring = Ring(P, [Buf(), Buf()])
            uring = Ring(P, [Buf(), Buf()])
            wgv = w_gate.rearrange("(k p) n -> p k n", p=128)
            wuv = w_up.rearrange("(k p) n -> p k n", p=128)
            NB = NFF // 2
            wl = {}

            def loadw(fb):
                gb, gsem = gring.next()
                ub, usem = uring.next()
                P.dma("gpsimd", gsem, wg[fb % 2][:], wgv[:, :, fb * 256:(fb + 1) * 256], writes=[gb])
                P.dma("gpsimd", usem, wu[fb % 2][:], wuv[:, :, fb * 256:(fb + 1) * 256], writes=[ub])
                wl[fb] = (gb, ub)

            loadw(0)
            loadw(1)
            nn_ = 0
            for fb in range(NB):
                gb, ub = wl[fb]
                for ft in range(2):
                    fi = fb * 2 + ft
                    for tc in range(2):
                        b2 = nn_ % 2
                        nn_ += 1
                        hb_ = Bh2[tc * 4:(tc + 1) * 4]
                        for k in range(KC):
                            P.op("tensor", lambda e, b2=b2, k=k, fb=fb, ft=ft, tc=tc: e.matmul(
                                pg[b2][:], lhsT=wg[fb % 2][:, k, ft * 128:(ft + 1) * 128], rhs=h2T[:, k, tc * 512:(tc + 1) * 512],
                                start=(k == 0), stop=(k == KC - 1)),
                                reads=[gb] + hb_, writes=[Bpg[b2]] if k == 0 else [], mark=(k == KC - 1))
                        for k in range(KC):
                            P.op("tensor", lambda e, b2=b2, k=k, fb=fb, ft=ft, tc=tc: e.matmul(
                                pu[b2][:], lhsT=wu[fb % 2][:, k, ft * 128:(ft + 1) * 128], rhs=h2T[:, k, tc * 512:(tc + 1) * 512],
                                start=(k == 0), stop=(k == KC - 1)),
                                reads=[ub] + hb_, writes=[Bpu[b2]] if k == 0 else [], mark=(k == KC - 1))
                        P.op("scalar", lambda e, b2=b2: e.activation(out=sg[b2][:], in_=pg[b2][:], func=AF.Silu),
                             reads=[Bpg[b2]], writes=[Bsg[b2]])
                        P.op("vector", lambda e, b2=b2, fi=fi, tc=tc: e.tensor_tensor(
                            out=aT[:, fi, tc * 512:(tc + 1) * 512], in0=sg[b2][:], in1=pu[b2][:], op=ALU.mult),
                            reads=[Bsg[b2], Bpu[b2]], writes=[B_aT[fi]])
                if fb + 2 < NB:
                    loadw(fb + 2)
            P.run_block()
            gring.release()
            uring.release()
    ssqG = sb("ssqG", [128, 32], F32, ffn_stack)
    BssqG = [Buf() for _ in range(8)]
    with ExitStack() as st:
        wd = [sb("wd%d" % i, [128, NFF, 512], BF16, st) for i in range(2)]
        fst = [sb("fst%d" % i, [128, 512], F32, st) for i in range(3)]
        junkd = sb("junkd", [128, 512], BF16, st)
        acc = [ps("accG%d" % i, [128, 512], F32, st) for i in range(4)]
        Bacc = [Buf() for _ in range(4)]
        Bjk = Buf()
        wring = Ring(P, [Buf(), Buf()])
        fring = Ring(P, [Buf(), Buf(), Buf()])
        wdv = w_down.rearrange("(k p) n -> p k n", p=128)
        P.op("vector", lambda e: e.memset(ssqG[:], 0.0), writes=BssqG)
        wbs = []
        for cb in range(2):
            wb, wsem = wring.next()
            P.dma("gpsimd", wsem, wd[cb % 2][:, 0:22, :], wdv[:, 0:22, cb * 512:(cb + 1) * 512], writes=[wb])
            P.dma("gpsimd", wsem, wd[cb % 2][:, 22:44, :], wdv[:, 22:44, cb * 512:(cb + 1) * 512], writes=[wb])
            wbs.append(wb)
        na = 0
        for cb in range(4):
            wb = wbs[cb]
            for tt in range(8):
                ab = na % 4
                na += 1
                for k in range(NFF):
                    P.op("tensor", lambda e, ab=ab, k=k, tt=tt, cb=cb: e.matmul(
                        acc[ab][:], lhsT=aT[:, k, tt * 128:(tt + 1) * 128], rhs=wd[cb % 2][:, k, :],
                        start=(k == 0), stop=(k == NFF - 1)),
                        reads=[B_aT[k], wb], writes=[Bacc[ab]] if k == 0 else [], mark=(k == NFF - 1))
                fb_, fsem = fring.next()
                fi_ = (fring.k - 1) % 3
                P.op("vector", lambda e, ab=ab, fi_=fi_: e.tensor_copy(out=fst[fi_][:], in_=acc[ab][:]),
                     reads=[Bacc[ab]], writes=[fb_])
                P.op("scalar", lambda e, fi_=fi_, tt=tt, cb=cb: e.activation(
                    out=junkd[:], in_=fst[fi_][:], func=AF.Square, accum_out=ssqG[:, tt * 4 + cb:tt * 4 + cb + 1]),
                    reads=[fb_], writes=[Bjk, BssqG[tt]])
                P.dma("sync", fsem, ff_scr[tt * 128:(tt + 1) * 128, cb * 512:(cb + 1) * 512], fst[fi_][:], reads=[fb_])
            if cb + 2 < 4:
                wb2, wsem = wring.next()
                P.dma("gpsimd", wsem, wd[cb % 2][:, 0:22, :], wdv[:, 0:22, (cb + 2) * 512:(cb + 3) * 512], writes=[wb2])
                P.dma("gpsimd", wsem, wd[cb % 2][:, 22:44, :], wdv[:, 22:44, (cb + 2) * 512:(cb + 3) * 512], writes=[wb2])
                wbs.append(wb2)
        P.run_block(final_waits=[Ev(s_, s_.n) for s_ in fring.sems])
        wring.release()
        fring.release()
    with ExitStack() as st:
        fft = [sb("fft%d" % i, [128, D], F32, st) for i in range(2)]
        sems2 = P.get_dma_sems(2)
        ga, Bga = row_gain("G", 5, g_ffn_post, st, sems2)
        ffring = Ring(P, [Buf(), Buf()])
        Bff = []
        xr = None
        srcB = [None] * 8

        def src_tiles(tt):
            return fft[tt % 2][:]

        xrr = [sb("xrG%d" % i, [128, D], F32, st) for i in range(2)]
        og = [sb("ogG%d" % i, [128, D], F32, st) for i in range(2)]
        s8 = sb("s8G", [128, 8], F32, st)
        Bs8 = [Buf() for _ in range(8)]
        xring = Ring(P, [Buf(), Buf()])
        oring = Ring(P, [Buf(), Buf()])
        for tt in range(8):
            fb_, fsem = ffring.next()
            xb, xsem = xring.next()
            ob, osem = oring.next()
            b2 = tt % 2
            P.dma("sync", fsem, fft[b2][:], ff_scr[tt * 128:(tt + 1) * 128, :], writes=[fb_])
            P.dma("sync", xsem, xrr[b2][:], x1_scr[tt * 128:(tt + 1) * 128, :], writes=[xb])
            P.op("vector", lambda e, tt=tt: e.tensor_reduce(out=s8[:, tt:tt + 1], in_=ssqG[:, tt * 4:tt * 4 + 4], axis=AX.X, op=ALU.add),
                 reads=[BssqG[tt]], writes=[Bs8[tt]])
            P.op("scalar", lambda e, tt=tt: e.activation(out=s8[:, tt:tt + 1], in_=s8[:, tt:tt + 1], func=AF.Sqrt,
                                                         scale=1.0 / D, bias=epsT[:]),
                 reads=[Bs8[tt], B_eps], writes=[Bs8[tt]])
            P.op("vector", lambda e, tt=tt: e.reciprocal(out=s8[:, tt:tt + 1], in_=s8[:, tt:tt + 1]),
                 reads=[Bs8[tt]], writes=[Bs8[tt]])
            P.op("vector", lambda e, tt=tt, b2=b2: e.scalar_tensor_tensor(
                out=og[b2][:], in0=fft[b2][:], scalar=s8[:, tt:tt + 1], in1=ga[:], op0=ALU.mult, op1=ALU.mult),
                reads=[fb_, Bs8[tt], Bga], writes=[ob])
            P.op("vector", lambda e, b2=b2: e.tensor_tensor(out=og[b2][:], in0=og[b2][:], in1=xrr[b2][:], op=ALU.add),
                 reads=[xb, ob], writes=[ob])
            P.dma("sync", osem, out_own[tt * 128:(tt + 1) * 128, :], og[b2][:], reads=[ob])
        P.run_block(final_waits=[Ev(s_, s_.n) for s_ in oring.sems])
    ffn_stack.close()
    top.close()
    return nc


def host_constants(r):
    cst = np.zeros((128, 8), np.float32)
    for p in range(128):
        d = p % 64
        if d < 16:
            cst[p, 0] = np.float32(THETA) ** np.float32(-((2 * (d % 8)) / 16.0))
        if p < 32:
            cst[p, 1] = np.float32(THETA) ** np.float32(-((2 * (p % 16)) / 32.0))
    rm = np.zeros((2, 128, 128), np.float32)
    for m in range(128):
        d = m % 64
        base = m - d
        if d < 8:
            rm[0, base + d + 8, m] = -1.0
        elif d < 16:
            rm[0, base + d - 8, m] = 1.0
        if m < 16:
            rm[1, m + 16, m] = -1.0
        elif m < 32:
            rm[1, m - 16, m] = 1.0
    ident = np.eye(128, dtype=np.float32)
    dmask = np.zeros((128, 32), np.float32)
    for p in range(128):
        dmask[p, p % 32] = 1.0
    bis = np.zeros((128, 2 * NBIS), np.float32)
    for it in range(NBIS):
        bis[:, it] = 2.0 ** (-(it + 1))
        bis[:, NBIS + it] = 2.0 ** (-(it + 2)) if it < NBIS - 1 else 2.0 ** (-(it + 1))
    blocks = slot_blocks(r)
    maskT = np.zeros((128, 32, 128), np.float32)
    maskadd = np.zeros((128, 8, 512), np.float32)
    kk = np.arange(128)[:, None]
    qq = np.arange(128)[None, :]
    for i in range(8):
        n = slot_ntiles(i)
        for j in range(4):
            kt = n - 4 + j
            if kt < blocks[i]:
                m = np.ones((128, 128), np.float32)
            elif kt == blocks[i]:
                m = (kk <= qq).astype(np.float32)
            else:
                m = np.zeros((128, 128), np.float32)
            maskT[:, i * 4 + j, :] = m
            maskadd[:, i, j * 128:(j + 1) * 128] = (m.T - 1.0) * 1.0e30
    return dict(cst_f32=cst, cst_rm=rm, cst_ident=ident, cst_dmask=dmask, cst_bis=bis,
                cst_maskT=maskT, cst_maskadd=maskadd)


def make_in_maps(inputs):
    x = np.asarray(inputs["x"], np.float32)
    c = np.asarray(inputs["c"], np.float32)
    pos = np.asarray(inputs["positions"], np.int32)
    shared = {}
    for k in ["w_ada", "b_ada", "g_attn_pre", "g_attn_post", "g_ffn_pre", "g_ffn_post", "w_in",
              "lambda_q1", "lambda_k1", "lambda_q2", "lambda_k2", "g_diff_sub", "w_o", "w_gate", "w_up", "w_down"]:
        a = np.asarray(inputs[k], np.float32)[0]
        if a.ndim == 1:
            a = a[None, :]
        shared[k] = np.ascontiguousarray(a)
    in_maps = []
    for core in range(8):
        b, r = divmod(core, 4)
        blocks = slot_blocks(r)
        idx = np.concatenate([np.arange(bl * 128, (bl + 1) * 128) for bl in blocks])
        m = dict(shared)
        m["x_all"] = np.ascontiguousarray(x[b])
        m["x_own"] = np.ascontiguousarray(x[b][idx])
        m["pos_all"] = np.ascontiguousarray(pos[b][None, :])
        m["pos_own"] = np.ascontiguousarray(pos[b][idx][None, :])
        m["c_in"] = np.ascontiguousarray(c[b][None, :])
        m.update(host_constants(r))
        in_maps.append(m)
    return in_maps


def kernel(**inputs):
    nc = build_program()
    in_maps = make_in_maps(inputs)
    res = run_bass_kernel_spmd(nc, in_maps, core_ids=list(range(8)))
    out = np.zeros((2, S, D), np.float32)
    for core in range(8):
        b, r = divmod(core, 4)
        blocks = slot_blocks(r)
        o = res.results[core]["out_own"]
        for i, bl in enumerate(blocks):
            out[b, bl * 128:(bl + 1) * 128] = o[i * 128:(i + 1) * 128]
    return out
```
